# Optimizing a Trainium2 kernel written in Bass

```python
import math
import jax, jax.numpy as jnp
from jax import lax
import numpy as np

D_MODEL = 1024
BATCH = 2
SEQ = 16384
DEPTH = 2

RG_WIDTH = 512
RG_BLOCKS = 8
RG_BLOCK_DIM = RG_WIDTH // RG_BLOCKS
CONV_WIDTH = 4
RG_C = 8.0
GLA_HEADS = 4
GLA_DK = 64
GLA_DV = 128
GLA_RANK = 16
GLA_GATE_NORMALIZER = 16.0
HG_HEADS = 4
HG_DK = 128
HG_DV = 128
HG_F_MIN = 1e-30
N_BRANCHES = 3
BRANCH_WIDTH = 512
CHUNK = 64
D_FF = 2816
N_EXPERTS = 8
TOP_K = 2
D_FF_EXPERT = 3584
MOE_BLOCK = 512
N_DENSE = (DEPTH + 1) // 2
N_MOE = DEPTH // 2
ALPHA = (2 * DEPTH) ** 0.25
BETA = (8 * DEPTH) ** -0.25
LN_EPS = 1e-5
RMS_EPS = 1e-6

SPLIT_SIZES = (RG_WIDTH,
               GLA_HEADS * GLA_DK, GLA_HEADS * GLA_DK, GLA_HEADS * GLA_DV, GLA_RANK, GLA_HEADS * GLA_DV,
               HG_HEADS * HG_DK, HG_HEADS * HG_DK, HG_HEADS * HG_DV, HG_HEADS * HG_DV,
               N_BRANCHES * D_MODEL)
D_IN = sum(SPLIT_SIZES)
SPLIT_POINTS = tuple(sum(SPLIT_SIZES[:i + 1]) for i in range(len(SPLIT_SIZES) - 1))

kernel_name = 'hybrid_rglru_gla_hgrn2_moe_deepnorm'


def layer_norm(x, g, b):
    xf = x.astype(jnp.float32)
    mu = jnp.mean(xf, axis=-1, keepdims=True)
    var = jnp.mean(jnp.square(xf - mu), axis=-1, keepdims=True)
    y = (xf - mu) * lax.rsqrt(var + LN_EPS) * g.astype(jnp.float32) + b.astype(jnp.float32)
    return y.astype(x.dtype)


def rms_norm(x, w):
    xf = x.astype(jnp.float32)
    return xf * lax.rsqrt(jnp.mean(xf * xf, axis=-1, keepdims=True) + RMS_EPS) * w.astype(jnp.float32)


def chunked_gated_linear_attention(q, k, v, log_g, scale):
    B, S, H, K = q.shape
    V = v.shape[-1]
    N = S // CHUNK

    def chunks(t):
        return t.astype(jnp.float32).reshape(B, N, CHUNK, H, t.shape[-1]).transpose(1, 0, 3, 2, 4)

    qc, kc, vc = chunks(q * scale), chunks(k), chunks(v)
    G = jnp.cumsum(chunks(log_g), axis=3)
    causal = jnp.tril(jnp.ones((CHUNK, CHUNK), dtype=bool))[:, :, None]

    def step(state, inp):
        qi, ki, vi, Gi = inp
        o_inter = jnp.einsum('bhck,bhkv->bhcv', qi * jnp.exp(Gi), state)
        diff = jnp.where(causal, Gi[:, :, :, None, :] - Gi[:, :, None, :, :], 0.0)
        decay = jnp.where(causal, jnp.exp(diff), 0.0)
        scores = jnp.einsum('bhck,bhsk,bhcsk->bhcs', qi, ki, decay)
        o_intra = jnp.einsum('bhcs,bhsv->bhcv', scores, vi)
        G_last = Gi[:, :, -1:, :]
        k_dec = ki * jnp.exp(G_last - Gi)
        state = state * jnp.exp(G_last[:, :, 0, :, None]) + jnp.einsum('bhsk,bhsv->bhkv', k_dec, vi)
        return state, o_inter + o_intra

    state0 = jnp.zeros((B, H, K, V), jnp.float32)
    _, o = lax.scan(step, state0, (qc, kc, vc, G))
    return o.transpose(1, 0, 3, 2, 4).reshape(B, S, H, V)


def rglru_branch(u, conv_w, conv_b, w_r, b_r, w_i, b_i, lam):
    B, S, W = u.shape
    u_pad = jnp.pad(u, ((0, 0), (CONV_WIDTH - 1, 0), (0, 0)))
    c = conv_b
    for j in range(CONV_WIDTH):
        c = c + u_pad[:, j:j + S] * conv_w[j]
    cb = c.reshape(B, S, RG_BLOCKS, RG_BLOCK_DIM)
    r = jax.nn.sigmoid(jnp.einsum('bsgi,gij->bsgj', cb, w_r).reshape(B, S, W) + b_r).astype(jnp.float32)
    i = jax.nn.sigmoid(jnp.einsum('bsgi,gij->bsgj', cb, w_i).reshape(B, S, W) + b_i).astype(jnp.float32)
    log_a = -RG_C * r * jax.nn.softplus(-lam.astype(jnp.float32))
    a = jnp.exp(log_a)
    b = jnp.sqrt(jnp.maximum(-jnp.expm1(2.0 * log_a), 0.0)) * (i * c.astype(jnp.float32))

    def combine(left, right):
        return left[0] * right[0], right[0] * left[1] + right[1]

    _, h = lax.associative_scan(combine, (a, b), axis=1)
    return h.astype(u.dtype)


def gla_branch(q, k, v, a_low, g, w_a, b_a, norm_w):
    B, S, _ = q.shape
    log_alpha = jax.nn.log_sigmoid((a_low @ w_a + b_a).astype(jnp.float32)) / GLA_GATE_NORMALIZER
    o = chunked_gated_linear_attention(
        q.reshape(B, S, GLA_HEADS, GLA_DK), k.reshape(B, S, GLA_HEADS, GLA_DK),
        v.reshape(B, S, GLA_HEADS, GLA_DV), log_alpha.reshape(B, S, GLA_HEADS, GLA_DK), GLA_DK ** -0.5)
    o = rms_norm(o, norm_w).reshape(B, S, GLA_HEADS * GLA_DV)
    return (o * jax.nn.silu(g.astype(jnp.float32))).astype(q.dtype)


def hgrn2_branch(q, f, i, g, lower_bound, norm_w):
    B, S, _ = q.shape
    z = f.astype(jnp.float32)
    forget = lower_bound + (1.0 - lower_bound) * jax.nn.sigmoid(z)
    log_f = jnp.log(jnp.maximum(forget, HG_F_MIN))
    k = (1.0 - lower_bound) * jax.nn.sigmoid(-z)
    qs = jax.nn.silu(q.astype(jnp.float32))
    o = chunked_gated_linear_attention(
        qs.reshape(B, S, HG_HEADS, HG_DK), k.reshape(B, S, HG_HEADS, HG_DK),
        i.reshape(B, S, HG_HEADS, HG_DV), log_f.reshape(B, S, HG_HEADS, HG_DK), HG_DK ** -0.5)
    o = rms_norm(o.reshape(B, S, HG_HEADS * HG_DV), norm_w)
    return (o * jax.nn.sigmoid(g.astype(jnp.float32))).astype(q.dtype)


def token_mixer(x, w_in, conv_w, conv_b, rg_wr, rg_br, rg_wi, rg_bi, rg_lambda,
                gla_wa, gla_ba, gla_norm_w, lower_bound, hg_norm_w, w_branch, w_out):
    B, S, _ = x.shape
    proj = x @ w_in
    (rg_x, gq, gk, gv, ga, gg, hq, hf, hi, hg, gates) = jnp.split(proj, SPLIT_POINTS, axis=-1)
    y_rg = rglru_branch(rg_x, conv_w, conv_b, rg_wr, rg_br, rg_wi, rg_bi, rg_lambda)
    y_gla = gla_branch(gq, gk, gv, ga, gg, gla_wa, gla_ba, gla_norm_w)
    y_hg = hgrn2_branch(hq, hf, hi, hg, lower_bound, hg_norm_w)
    ys = jnp.stack([y_rg, y_gla, y_hg], axis=2)
    branch_out = jnp.einsum('bsnw,nwd->bsnd', ys, w_branch)
    merge = jax.nn.sigmoid(gates.reshape(B, S, N_BRANCHES, D_MODEL))
    mixed = jnp.sum(merge * branch_out, axis=2)
    return mixed @ w_out


def swiglu(x, w_gate, w_up, w_down):
    return (jax.nn.silu(x @ w_gate) * (x @ w_up)) @ w_down


def moe_swiglu(x, router_w, w_gate, w_up, w_down):
    B, S, D = x.shape
    T = B * S
    TK = T * TOP_K
    xf = x.reshape(T, D)
    logits = (xf @ router_w).astype(jnp.float32)
    top_logits, top_idx = lax.top_k(logits, TOP_K)
    top_w = jax.nn.softmax(top_logits, axis=-1)
    expert = top_idx.reshape(-1).astype(jnp.int32)
    token = jnp.repeat(jnp.arange(T, dtype=jnp.int32), TOP_K)
    weight = top_w.reshape(-1)
    order = jnp.argsort(expert, stable=True)
    e_sorted = expert[order]
    counts = jnp.bincount(expert, length=N_EXPERTS).astype(jnp.int32)
    padded = (counts + MOE_BLOCK - 1) // MOE_BLOCK * MOE_BLOCK
    start = jnp.cumsum(counts) - counts
    pad_end = jnp.cumsum(padded)
    pad_start = pad_end - padded
    dest = pad_start[e_sorted] + jnp.arange(TK, dtype=jnp.int32) - start[e_sorted]
    n_blocks = -(-TK // MOE_BLOCK) + N_EXPERTS
    P = n_blocks * MOE_BLOCK
    buf_tok = jnp.zeros((P,), jnp.int32).at[dest].set(token[order])
    buf_w = jnp.zeros((P,), jnp.float32).at[dest].set(weight[order])
    block_start = jnp.arange(n_blocks, dtype=jnp.int32) * MOE_BLOCK
    block_expert = jnp.minimum(jnp.searchsorted(pad_end, block_start, side='right'), N_EXPERTS - 1)
    xs = xf[buf_tok].reshape(n_blocks, MOE_BLOCK, D)

    def expert_block(args):
        xb, e = args
        return swiglu(xb, w_gate[e], w_up[e], w_down[e])

    ys = lax.map(expert_block, (xs, block_expert)).reshape(P, D)
    y = jnp.zeros((T, D), ys.dtype).at[buf_tok].add(ys * buf_w[:, None].astype(ys.dtype))
    return y.reshape(B, S, D).astype(x.dtype)


def setup_inputs(seed: int = 0) -> dict:
    key = jax.random.key(seed)
    ks = jax.random.split(key, 28)
    f32 = jnp.float32

    def dense(k, shape, fan_in, scale=1.0):
        return jax.random.normal(k, shape, f32) * (scale * fan_in ** -0.5)

    def small(k, shape):
        return 0.01 * jax.random.normal(k, shape, f32)

    def gain(k, shape):
        return 1.0 + 0.01 * jax.random.normal(k, shape, f32)

    hk = GLA_HEADS * GLA_DK
    a_pow = jax.random.uniform(ks[9], (DEPTH, RG_WIDTH), f32, 0.9, 0.999)
    a_base = a_pow ** (1.0 / RG_C)
    return {
        'x': jax.random.normal(ks[0], (BATCH, SEQ, D_MODEL), f32),
        'w_in': dense(ks[1], (DEPTH, D_MODEL, D_IN), D_MODEL),
        'conv_w': dense(ks[2], (DEPTH, CONV_WIDTH, RG_WIDTH), CONV_WIDTH),
        'conv_b': small(ks[3], (DEPTH, RG_WIDTH)),
        'rg_wr': dense(ks[4], (DEPTH, RG_BLOCKS, RG_BLOCK_DIM, RG_BLOCK_DIM), RG_BLOCK_DIM),
        'rg_br': small(ks[5], (DEPTH, RG_WIDTH)),
        'rg_wi': dense(ks[6], (DEPTH, RG_BLOCKS, RG_BLOCK_DIM, RG_BLOCK_DIM), RG_BLOCK_DIM),
        'rg_bi': small(ks[7], (DEPTH, RG_WIDTH)),
        'rg_lambda': jnp.log(a_base) - jnp.log1p(-a_base),
        'gla_wa': dense(ks[8], (DEPTH, GLA_RANK, hk), GLA_RANK),
        'gla_ba': small(ks[10], (DEPTH, hk)),
        'gla_norm_w': gain(ks[11], (DEPTH, GLA_DV)),
        'hg_lb_logits': 0.1 * jax.random.normal(ks[12], (DEPTH, HG_HEADS * HG_DK), f32),
        'hg_norm_w': gain(ks[13], (DEPTH, HG_HEADS * HG_DV)),
        'w_branch': dense(ks[14], (DEPTH, N_BRANCHES, BRANCH_WIDTH, D_MODEL), BRANCH_WIDTH, BETA),
        'w_out': dense(ks[15], (DEPTH, D_MODEL, D_MODEL), D_MODEL, BETA),
        'ln1_g': gain(ks[16], (DEPTH, D_MODEL)),
        'ln1_b': small(ks[17], (DEPTH, D_MODEL)),
        'ln2_g': gain(ks[18], (DEPTH, D_MODEL)),
        'ln2_b': small(ks[19], (DEPTH, D_MODEL)),
        'ffn_wg': dense(ks[20], (N_DENSE, D_MODEL, D_FF), D_MODEL),
        'ffn_wu': dense(ks[21], (N_DENSE, D_MODEL, D_FF), D_MODEL),
        'ffn_wd': dense(ks[22], (N_DENSE, D_FF, D_MODEL), D_FF, BETA),
        'router_w': dense(ks[23], (N_MOE, D_MODEL, N_EXPERTS), D_MODEL),
        'moe_wg': dense(ks[24], (N_MOE, N_EXPERTS, D_MODEL, D_FF_EXPERT), D_MODEL),
        'moe_wu': dense(ks[25], (N_MOE, N_EXPERTS, D_MODEL, D_FF_EXPERT), D_MODEL),
        'moe_wd': dense(ks[26], (N_MOE, N_EXPERTS, D_FF_EXPERT, D_MODEL), D_FF_EXPERT, BETA),
    }


def reference(x, w_in, conv_w, conv_b, rg_wr, rg_br, rg_wi, rg_bi, rg_lambda,
              gla_wa, gla_ba, gla_norm_w, hg_lb_logits, hg_norm_w, w_branch, w_out,
              ln1_g, ln1_b, ln2_g, ln2_b, ffn_wg, ffn_wu, ffn_wd,
              router_w, moe_wg, moe_wu, moe_wd):
    lb_w = jax.nn.softmax(hg_lb_logits.astype(jnp.float32), axis=0)
    lower_bounds = jnp.cumsum(lb_w, axis=0) - lb_w[0]
    for l in range(DEPTH):
        h = token_mixer(x, w_in[l], conv_w[l], conv_b[l], rg_wr[l], rg_br[l], rg_wi[l], rg_bi[l],
                        rg_lambda[l], gla_wa[l], gla_ba[l], gla_norm_w[l], lower_bounds[l],
                        hg_norm_w[l], w_branch[l], w_out[l])
        x = layer_norm(ALPHA * x + h, ln1_g[l], ln1_b[l])
        j = l // 2
        if l % 2 == 0:
            f = swiglu(x, ffn_wg[j], ffn_wu[j], ffn_wd[j])
        else:
            f = moe_swiglu(x, router_w[j], moe_wg[j], moe_wu[j], moe_wd[j])
        x = layer_norm(ALPHA * x + f, ln2_g[l], ln2_b[l])
    return x
```

```python
import numpy as np
from contextlib import ExitStack
import concourse.bass as bass
import concourse.mybir as mybir
from concourse.bass_utils import run_bass_kernel_spmd

F32 = mybir.dt.float32
BF16 = mybir.dt.bfloat16
AF = mybir.ActivationFunctionType
ALU = mybir.AluOpType
AX = mybir.AxisListType

P = 128
TT = 512
NT = 8
SEG = 4096
D = 1024
DIN = 7184
ALPHA = 4.0 ** 0.25
LN_EPS = 1e-5
RMS_EPS = 1e-6
NPV = 48
C_RG, C_GQ, C_GK, C_GV, C_GA, C_GG, C_HQ, C_HF, C_HI, C_HG, C_GATES = 0, 512, 768, 1024, 1536, 1552, 2064, 2576, 3088, 3600, 4112
ST_ROWS = 7 * P
NTF = 32
NPRE = 24

SAME_ENG_SYNC = True
BULK_FS = 1 << 30
N_SP_SEMS = 24
N_POOL_SEMS = 12


class Sched:
    def __init__(self):
        self.ops = []
        self.lastw = {}
        self.rd = {}

    def add(self, eng, fn, reads=(), writes=(), dma=False, fs=0):
        i = len(self.ops)
        deps = set()
        for k in reads:
            w = self.lastw.get(k)
            if w is not None:
                deps.add(w)
        for k in writes:
            w = self.lastw.get(k)
            if w is not None:
                deps.add(w)
            r = self.rd.get(k)
            if r:
                deps.update(r[0].values())
                deps.update(r[1])
        self.ops.append([eng, fn, deps, dma, bool(dma), None, fs])
        for k in reads:
            r = self.rd.setdefault(k, ({}, []))
            if dma:
                r[1].append(i)
            else:
                r[0][eng] = i
        for k in writes:
            self.lastw[k] = i
            self.rd[k] = ({}, [])
        return i

    def finalize(self, sems):
        ops = self.ops
        for i, op in enumerate(ops):
            for d in op[2]:
                dop = ops[d]
                if (not dop[3]) and dop[0] == op[0] and (not op[3]):
                    if dop[0] == "pe" or not SAME_ENG_SYNC or (dop[6] >= BULK_FS and dop[0] in ("act", "dve")):
                        continue
                dop[4] = True
        cnt = {e: 0 for e in ("pe", "act", "dve", "pool")}
        rr = {"sp": 0, "pool": 0}
        semcum = {}
        semlast = {}
        for i, op in enumerate(ops):
            if not op[4]:
                continue
            if op[3]:
                q = op[0]
                pool = sems["dma_" + q]
                s = pool[rr[q] % len(pool)]
                rr[q] += 1
                prev = semlast.get(id(s))
                if prev is not None:
                    op[2].add(prev)
                semcum[id(s)] = semcum.get(id(s), 0) + 16
                semlast[id(s)] = i
                op[5] = (s, semcum[id(s)], 16)
            else:
                cnt[op[0]] += 1
                op[5] = (sems[op[0]], cnt[op[0]], 1)

    def emit(self, engname, eng):
        ops = self.ops
        waited = {}
        for i, op in enumerate(ops):
            if op[0] != engname:
                continue
            need = {}
            for d in op[2]:
                dop = ops[d]
                sg = dop[5]
                if sg is None:
                    continue
                if (not dop[3]) and dop[0] == engname and (not op[3]):
                    if engname == "pe" or not SAME_ENG_SYNC or (dop[6] >= BULK_FS and engname in ("act", "dve")):
                        continue
                k = id(sg[0])
                if waited.get(k, 0) >= sg[1]:
                    continue
                if k not in need or need[k][1] < sg[1]:
                    need[k] = sg
            for k, sg in need.items():
                eng.wait_ge(sg[0], sg[1])
                waited[k] = sg[1]
            if op[1] is None:
                continue
            inst = op[1](eng)
            if op[5] is not None:
                inst.then_inc(op[5][0], op[5][2])


class Ring:
    def __init__(self, items):
        self.items = items
        self.free = list(range(len(items)))

    def get(self):
        assert self.free, "ring exhausted"
        return self.free.pop(0)

    def put(self, i):
        assert i not in self.free
        self.free.append(i)


class Builder:
    def __init__(self, phases, mode, debug=None):
        self.phases = phases
        self.mode = mode
        self.dbg = debug or {}
        self.nc = bass.Bass("TRN2", target_bir_lowering=False)
        self.S = Sched()
        self.es = ExitStack()
        self.dram = {}
        self.conv_done = set()
        self.outputs = []
        self.out_dmas = []

    def din(self, name, shape, dt=F32):
        t = self.nc.dram_tensor(name, list(shape), dt, kind="ExternalInput").ap()
        self.dram[name] = t
        return t

    def dout(self, name, shape, dt=F32):
        t = self.nc.dram_tensor(name, list(shape), dt, kind="ExternalOutput").ap()
        self.dram[name] = t
        self.outputs.append(name)
        return t

    def dint(self, name, shape, dt=F32):
        t = self.nc.dram_tensor(name, list(shape), dt, kind="Internal").ap()
        self.dram[name] = t
        return t

    def sb(self, name, shape, dt=F32):
        return self.es.enter_context(self.nc.sbuf_tensor(name, list(shape), dt))

    def dump(self, name, ap, shape, keys, dt=F32):
        t = self.dout(name, shape, dt)
        idx = tuple(slice(None) for _ in shape)
        self.dma("sp", t[idx], ap, keys, [("out", name)])
        self._outk.append(name)

    def op(self, eng, fn, r=(), w=(), dma=False, fs=0):
        return self.S.add(eng, fn, r, w, dma, fs)

    @staticmethod
    def fsz(ap):
        n = 1
        for d_ in ap.shape[1:]:
            n *= int(d_)
        return n

    def dma(self, q, out, in_, r, w, **kw):
        return self.op(q, lambda e: e.dma_start(out=out, in_=in_, **kw), r, w, dma=True)

    def mm(self, out, lhsT, rhs, start, stop, r, w):
        return self.op("pe", lambda e: e.matmul(out, lhsT, rhs, start=start, stop=stop), r, w)

    def mmk(self, out, pairs, r, w):
        n = len(pairs)

        def fn(e):
            inst = None
            for i, (a, b) in enumerate(pairs):
                inst = e.matmul(out, a, b, start=(i == 0), stop=(i == n - 1))
            return inst
        return self.op("pe", fn, r, w)

    def act(self, out, in_, func, r, w, bias=None, scale=None, accum_out=None):
        kw = {}
        if bias is not None:
            kw["bias"] = bias
        if scale is not None:
            kw["scale"] = scale
        if accum_out is not None:
            kw["accum_out"] = accum_out
        return self.op("act", lambda e: e.activation(out=out, in_=in_, func=func, **kw), r, w, fs=(0 if accum_out is not None else self.fsz(out)))

    def tt(self, out, in0, in1, op, r, w, eng="dve"):
        return self.op(eng, lambda e: e.tensor_tensor(out=out, in0=in0, in1=in1, op=op), r, w, fs=self.fsz(out))

    def ts(self, out, in0, s1, s2, op0, op1, r, w, eng="dve"):
        if op1 is None:
            return self.op(eng, lambda e: e.tensor_scalar(out=out, in0=in0, scalar1=s1, scalar2=None, op0=op0), r, w, fs=self.fsz(out))
        return self.op(eng, lambda e: e.tensor_scalar(out=out, in0=in0, scalar1=s1, scalar2=s2, op0=op0, op1=op1), r, w, fs=self.fsz(out))

    def stt(self, out, in0, scalar, in1, op0, op1, r, w):
        return self.op("dve", lambda e: e.scalar_tensor_tensor(out=out, in0=in0, scalar=scalar, in1=in1, op0=op0, op1=op1), r, w, fs=self.fsz(out))

    def cp(self, eng, out, in_, r, w):
        if eng == "act":
            return self.op("act", lambda e: e.copy(out=out, in_=in_), r, w, fs=self.fsz(out))
        return self.op(eng, lambda e: e.tensor_copy(out=out, in_=in_), r, w, fs=self.fsz(out))

    def wconv(self, name, src3, kc, n):
        if name in self.dram:
            return self.dram[name]
        t = self.dint(name, [P, kc, n], BF16)
        self.dma("pool", t[:, :, :], src3, r=(), w=[("dram", name)])
        return t

    def slab_load(self, name, kc, n):
        i = self.slabs.get()
        dst = self.slab_t[i][:, 0:kc, 0:n]
        self.dma("sp", dst, self.dram[name][:, :, :], r=[("dram", name)], w=[("slab", i)])
        return i

    def li(self, l):
        return l if self.mode == "fused" else 0

    def win_src(self, l, c0, n):
        return self.dram["w_in"][self.li(l), :, c0:c0 + n].rearrange("(kc p) n -> p kc n", p=P)

    def build(self):
        nc = self.nc
        ph = self.phases
        fused = self.mode == "fused"
        if fused:
            self.din("xlay", [NTF * TT, D])
            self.din("keep", [P, NTF])
        elif "L0P1" in ph or "L0P2" in ph:
            self.din("x", [SEG, D])
            self.din("halo0", [4, D])
        if not fused:
            self.din("masks", [P, 8])
            self.din("hmask", [P, 8])
        self.din("consts", [P, 1024])
        self.din("pvec", [P, 2 * NPV])
        self.din("wbd", [2, 2, P, 4, P])
        nl = 2 if fused else 1
        need = {"w_in": [nl, D, DIN], "gla_wa": [2, 16, 256]}
        if fused or "L0P2" in ph or "L1P2" in ph:
            need.update({"w_branch": [nl, 3, 512, D], "w_out": [nl, D, D], "ln1_g": [2, D], "ln1_b": [2, D], "ln2_g": [2, D], "ln2_b": [2, D]})
        if fused or "L0P2" in ph:
            need.update({"ffn_wg": [1, D, 2816], "ffn_wu": [1, D, 2816], "ffn_wd": [1, 2816, D]})
        if fused or "L1P2" in ph:
            need.update({"router_w": [1, D, 8], "moe_wg": [1, 8, D, 3584], "moe_wu": [1, 8, D, 3584], "moe_wd": [1, 8, 3584, D]})
        else:
            need.update({"router_w": [1, D, 8]})
        for nm, shp in need.items():
            self.din(nm, shp)
        if fused:
            self.dint("r1", [NTF * TT, D])
            self.dout("y", [SEG, D])
        else:
            if "L0P1" in ph:
                self.dout("st0", [ST_ROWS, P])
            if "L0P2" in ph:
                self.din("st0_all", [8 * ST_ROWS, P])
                self.dout("r1", [SEG, D])
            if "L1P1" in ph:
                self.din("r1", [SEG, D])
                self.din("hl_all", [32, D])
                self.dout("st1", [ST_ROWS, P])
            if "L1P2" in ph:
                self.din("r1", [SEG, D])
                self.din("hl_all", [32, D])
                self.din("st1_all", [8 * ST_ROWS, P])
                self.dout("y", [SEG, D])

        sb = self.sb
        self.consts = sb("consts_sb", [P, 1024])
        self.ident_bf = sb("ident_bf", [P, P], BF16)
        self.causal = sb("causal", [P, 2, 256], BF16)
        self.ones_g = sb("ones_g", [P, P], BF16)
        self.ones_h = sb("ones_h", [P, P], BF16)
        self.pv = sb("pv", [P, 2 * NPV])
        self.dv = sb("dv", [P, 2, 32])
        self.keep = sb("keep_sb", [P, NTF])
        self.mk = sb("mk", [P, 8])
        self.hm = sb("hm", [P, 8])
        self.lnp = sb("lnp", [P, 4, D])
        self.wbd = sb("wbd_bf", [P, 2, 4, P], BF16)
        self.wa = sb("wa_bf", [16, 256], BF16)
        self.rw = sb("rw", [P, 8, 8])
        self.St = sb("S_state", [P, 6, P])
        self.Sbf = sb("S_bf", [P, 2, 6, P], BF16)
        self.dch = sb("dch", [P, 6, 8])
        self.gsum = sb("gsum", [P, 6])
        self.gtmp = sb("gtmp", [P, 6])
        self.rsum = sb("rsum", [P, 4])
        self.rtmp = sb("rtmp", [P, 4])
        self.hc = sb("hcarry", [P, 4])
        if not fused:
            self.misc = sb("misc", [P, P])
            self.stl = sb("stl", [P, 7, P])
            self.deff = sb("deff", [P, 16])
            self.hal = sb("hal", [4, D])
            self.halj = sb("halj", [4, D])
        self.xTh = sb("xTh", [P, 8, 4], BF16)
        self.xtok_b = [sb("xtokA", [P, 4, D])] + ([sb("xtokB", [P, 4, D])] if fused else [])
        self.xi = 0
        self.xtok = self.xtok_b[0]
        self.xT = sb("xT", [P, 8, TT], BF16)
        self.x1T = sb("x1T", [P, 8, TT], BF16)
        self.slab_t = [sb(f"slab{i}", [P, 8, 512], BF16) for i in range(4 if fused else 3)]
        self.slabs = Ring(self.slab_t)
        self.uext = sb("uext", [P, 4, 4 + TT])
        self.yT = sb("yT", [P, 12, TT], BF16)
        self.qt = sb("qt", [P, 4, TT], BF16)
        self.kt = sb("kt", [P, 4, TT], BF16)
        self.kd = sb("kdT", [P, 4, TT], BF16)
        self.kdtok = sb("kdtok", [P, 4, 4, P], BF16)
        self.vtok = sb("vtok", [P, 4, 512], BF16)
        self.Asb = sb("Asb", [P, 2, 4, 256], BF16)
        self.mixT = sb("mixT", [P, 8, TT], BF16)
        self.hT = sb("hT", [P, 28, TT], BF16)
        self.alow = sb("alow", [16, TT], BF16)
        self.lnst = sb("lnst", [P, 4, 2, 6])
        self.lnmv = sb("lnmv", [P, 4, 2])
        self.lnr = sb("lnr", [P, 4, 2])
        self.rl = sb("rl", [P, 4, 8])
        self.rm8 = sb("rm8", [P, 4, 8])
        self.rwt = sb("rwt", [P, 4, 8])
        self.rsm = sb("rsm", [P, 4, 4])
        self.ring_t = [sb(f"ring{i}", [P, TT]) for i in range(7 if fused else 8)]
        self.ring = Ring(self.ring_t)
        self.ps_t = [self.es.enter_context(nc.psum_tensor(f"ps{i}", [P, 512], F32)) for i in range(8)]
        self.ps = Ring(self.ps_t)

        sem = lambda n: self.es.enter_context(nc.semaphore(n))
        self.sems = {e: sem("s_" + e) for e in ("pe", "act", "dve", "pool")}
        self.sems["dma_sp"] = [sem(f"d_sp{i}") for i in range(N_SP_SEMS)]
        self.sems["dma_pool"] = [sem(f"d_pl{i}") for i in range(N_POOL_SEMS)]

        self.prologue()
        if fused:
            for l in (0, 1):
                self.conv_win(l, ["rg", "ga", "qk", "gv", "hf", "hi"])
                self.conv_win(l, ["gg", "hq", "hg"] + [f"gt{i}" for i in range(6)])
                self.conv_layer_rest(l)
            self.fused_program()
        else:
            for p in ph:
                if p == "L0P1":
                    self.layer_pass(0, 1)
                elif p == "L0P2":
                    self.layer_pass(0, 2)
                elif p == "L1P1":
                    self.layer_pass(1, 1)
                elif p == "L1P2":
                    self.layer_pass(1, 2)
        self.op("sp", None, r=[("out", n) for n in self.out_keys()], w=())

        self.S.finalize(self.sems)
        S = self.S
        with nc.Block() as block:
            @block.sync
            def _(e):
                S.emit("sp", e)

            @block.tensor
            def _(e):
                S.emit("pe", e)

            @block.scalar
            def _(e):
                S.emit("act", e)

            @block.vector
            def _(e):
                S.emit("dve", e)

            @block.gpsimd
            def _(e):
                S.emit("pool", e)
        self.es.close()
        return nc

    def out_keys(self):
        return list(self._outk)

    def prologue(self):
        self._outk = []
        d = self.dram
        c = self.consts
        self.dma("sp", c[:], d["consts"][:, :], (), ["consts"])
        self.dma("sp", self.pv[:], d["pvec"][:, :], (), ["pv"])
        if self.mode == "fused":
            self.dma("sp", self.keep[:], d["keep"][:, :], (), ["keep"])
        else:
            self.dma("sp", self.mk[:], d["masks"][:, :], (), ["mk"])
            self.dma("sp", self.hm[:], d["hmask"][:, :], (), ["hm"])
        self.dma("sp", self.rw[:], d["router_w"][0].rearrange("(kc p) e -> p kc e", p=P), (), ["rw"])
        self.cp("dve", self.ident_bf[:], c[:, 0:128], ["consts"], ["ident"])
        self.op("dve", lambda e: e.memset(self.causal[:], 0.0), (), ["causal"])
        self.cp("dve", self.causal[0:64, 0, :], c[0:64, 640:896], ["consts", "causal"], ["causal"])
        self.cp("dve", self.causal[64:128, 1, :], c[64:128, 640:896], ["consts", "causal"], ["causal"])
        self.ts(self.ones_g[:], c[:, 896:1024], 1.0 / 128, None, ALU.mult, None, ["consts"], ["ones_g"])
        self.ts(self.ones_h[:], c[:, 896:1024], 1.0 / 512, None, ALU.mult, None, ["consts"], ["ones_h"])
        for l in (0, 1):
            pv = self.pv[:, l * NPV:(l + 1) * NPV]
            dv = self.dv[:, l, :]
            self.act(dv[:, 0:4], pv[:, 28:32], AF.Exp, ["pv"], [("dv", l, 0)], scale=-1.0)
            self.act(dv[:, 0:4], dv[:, 0:4], AF.Ln, [("dv", l, 0)], [("dv", l, 0)], bias=1.0)
            self.ts(dv[:, 4:8], dv[:, 0:4], -16.0, None, ALU.mult, None, [("dv", l, 0)], [("dv", l, 1)])
            self.ts(dv[:, 0:4], dv[:, 0:4], -8.0, None, ALU.mult, None, [("dv", l, 0), ("dv", l, 1)], [("dv", l, 0)])
            self.ts(dv[:, 8:10], pv[:, 32:34], -1.0, None, ALU.mult, None, ["pv"], [("dv", l, 2)])
            if l == 0:
                self.op("dve", lambda e, o=dv[:, 10:14]: e.memset(o, 0.0), (), [("dv", l, 3)])
            else:
                self.tt(dv[:, 10:14], pv[:, 39:43], pv[:, 35:39], ALU.subtract, ["pv"], [("dv", l, 3)])
                self.act(dv[:, 10:14], dv[:, 10:14], AF.Sigmoid, [("dv", l, 3)], [("dv", l, 3)])
            self.ts(dv[:, 14:18], dv[:, 10:14], -1.0, 1.0, ALU.mult, ALU.add, [("dv", l, 3)], [("dv", l, 4)])
            self.ts(dv[:, 18:22], dv[:, 10:14], -1.0, None, ALU.add, None, [("dv", l, 3)], [("dv", l, 5)])

    def conv_win(self, l, names):
        tab = {"rg": (C_RG, 512), "qk": (C_GQ, 512), "gv": (C_GV, 512), "ga": (C_GA, 16), "gg": (C_GG, 512),
               "hq": (C_HQ, 512), "hf": (C_HF, 512), "hi": (C_HI, 512), "hg": (C_HG, 512)}
        for i in range(6):
            tab[f"gt{i}"] = (C_GATES + 512 * i, 512)
        for nm in names:
            c0, n = tab[nm]
            self.wconv(f"win{l}_{nm}", self.win_src(l, c0, n), 8, n)

    def conv_layer_rest(self, l):
        d = self.dram
        for n in range(3):
            for dh in range(2):
                self.wconv(f"wbr{l}_{n}_{dh}", d["w_branch"][self.li(l), n, :, dh * 512:(dh + 1) * 512].rearrange("(kc p) n -> p kc n", p=P), 4, 512)
        for dh in range(2):
            self.wconv(f"wout{l}_{dh}", d["w_out"][self.li(l), :, dh * 512:(dh + 1) * 512].rearrange("(kc p) n -> p kc n", p=P), 8, 512)
        if l == 0:
            for g in range(6):
                n = 512 if g < 5 else 256
                self.wconv(f"fg_{g}", d["ffn_wg"][0, :, g * 512:g * 512 + n].rearrange("(kc p) n -> p kc n", p=P), 8, n)
                self.wconv(f"fu_{g}", d["ffn_wu"][0, :, g * 512:g * 512 + n].rearrange("(kc p) n -> p kc n", p=P), 8, n)
            for dh in range(2):
                for g in range(6):
                    nf = 4 if g < 5 else 2
                    self.wconv(f"fd_{g}_{dh}", d["ffn_wd"][0, g * 512:g * 512 + nf * P, dh * 512:(dh + 1) * 512].rearrange("(fc p) n -> p fc n", p=P), nf, 512)
        else:
            for e in range(8):
                for g in range(7):
                    self.wconv(f"mg_{e}_{g}", d["moe_wg"][0, e, :, g * 512:(g + 1) * 512].rearrange("(kc p) n -> p kc n", p=P), 8, 512)
                    self.wconv(f"mu_{e}_{g}", d["moe_wu"][0, e, :, g * 512:(g + 1) * 512].rearrange("(kc p) n -> p kc n", p=P), 8, 512)
                for dh in range(2):
                    for g in range(7):
                        self.wconv(f"md_{e}_{g}_{dh}", d["moe_wd"][0, e, g * 512:(g + 1) * 512, dh * 512:(dh + 1) * 512].rearrange("(fc p) n -> p fc n", p=P), 4, 512)

    def layer_consts(self, l, full):
        d = self.dram
        R = self.ring
        i0 = R.get()
        wa_f = self.ring_t[i0][0:16, 0:256]
        self.dma("sp", wa_f, d["gla_wa"][l], (), [("ring", i0)])
        self.cp("dve", self.wa[:], wa_f, [("ring", i0)], ["wa"])
        R.put(i0)
        for j in range(2):
            i1 = R.get()
            wbd_f = self.ring_t[i1][:].rearrange("p (c m) -> p c m", m=P)
            self.dma("sp", wbd_f, d["wbd"][l, j], (), [("ring", i1)])
            self.cp("dve", self.wbd[:, j], wbd_f, [("ring", i1)], [("wbd", j)])
            R.put(i1)
        if full:
            for j, nm in enumerate(("ln1_g", "ln1_b", "ln2_g", "ln2_b")):
                self.dma("sp", self.lnp[:, j, :], d[nm][l:l + 1, :].broadcast_to([P, D]), (), [("lnp", j)])

    def transpose_tok(self, src, src_keys, dstT, dst_key):
        for kc in range(8):
            b = self.ps.get()
            pf = self.ps_t[b]

            def fn(e, kc=kc, pf=pf):
                inst = None
                for blk in range(4):
                    inst = e.transpose(pf[:, blk * P:(blk + 1) * P], src[:, blk, kc * P:(kc + 1) * P], self.consts[:, 0:128])
                return inst
            self.op("pe", fn, list(src_keys) + ["consts"], [("ps", b)])
            eng = "act" if kc % 2 == 0 else "dve"
            self.cp(eng, dstT[:, kc, :], pf[:, 0:TT], [("ps", b)], [(dst_key, kc)])
            self.ps.put(b)

    def proj_fm(self, slab, xT, xkey, col0, M=P, N=TT, ncols=None):
        b = self.ps.get()
        st = self.slab_t[slab]
        out = self.ps_t[b][0:M, 0:N]
        pairs = [(st[:, kc, col0:col0 + M], xT[:, kc, 0:N]) for kc in range(8)]
        self.mmk(out, pairs, [("slab", slab)] + [(xkey, kc) for kc in range(8)], [("ps", b)])
        return b

    def proj_tm(self, slab, xT, xkey, blk, n=512):
        b = self.ps.get()
        st = self.slab_t[slab]
        out = self.ps_t[b][:, 0:n]
        pairs = [(xT[:, kc, blk * P:(blk + 1) * P], st[:, kc, 0:n]) for kc in range(8)]
        self.mmk(out, pairs, [("slab", slab)] + [(xkey, kc) for kc in range(8)], [("ps", b)])
        return b

    def layer_pass(self, l, pas):
        d = self.dram
        full = pas == 2
        fused = self.mode == "fused"
        res_in = d["x"] if l == 0 else d["r1"]
        self.resname = "x" if l == 0 else "r1"
        res_out = None
        if full:
            res_out = d["r1"] if l == 0 else d["y"]
        if pas == 1 or not fused:
            self.conv_win(l, ["rg", "ga", "qk", "gv", "hf", "hi"])
        if full:
            self.conv_win(l, ["gg", "hq", "hg"] + [f"gt{i}" for i in range(6)])
            self.conv_layer_rest(l)
        self.layer_consts(l, full)
        pv = self.pv[:, l * NPV:(l + 1) * NPV]
        dv = self.dv[:, l, :]
        DVK = [("dv", l, i) for i in range(6)] + ["pv"]

        z = lambda ap, keys: self.op("dve", lambda e: e.memset(ap, 0.0), (), keys)
        z(self.St[:], [("S", b) for b in range(6)])
        z(self.hc[:], ["hc"])
        if not full:
            z(self.gsum[:], ["gsum"])
            z(self.rsum[:], ["rsum"])
        else:
            self.combine_states(l)
        self.halo(l)

        if self.dbg.get("stop") == "pre":
            self.dump("dbg_S", self.St[:], [P, 6, P], [("S", b) for b in range(6)])
            return
        for t in range(self.dbg.get("ntiles", NT)):
            if self.tile(l, t, full, res_in, res_out, pv, dv, DVK) == "stop":
                return

        if not full:
            self.write_states(l, dv, DVK)
        elif l == 0 and (fused or True):
            pass

    def zero_states(self):
        z = lambda ap, keys: self.op("dve", lambda e: e.memset(ap, 0.0), (), keys)
        z(self.St[:], [("S", b) for b in range(6)])
        z(self.hc[:], ["hc"])
        z(self.gsum[:], ["gsum"])
        z(self.rsum[:], ["rsum"])
        z(self.uext[:, :, 0:4], [("uext", c) for c in range(4)])
        self.cp("act", self.Sbf[:, 0, :, :], self.St[:], [("S", b) for b in range(6)], [("Sbf", 0, b) for b in range(6)])
        self.halo_pending = False

    def mask_states(self, t, full):
        k = self.keep[:, t:t + 1]
        SK = [("S", b) for b in range(6)]
        self.ts(self.St[:], self.St[:], k, None, ALU.mult, None, SK + ["keep"], SK)
        self.ts(self.hc[:], self.hc[:], k, None, ALU.mult, None, ["hc", "keep"], ["hc"])
        UK = [("uext", c) for c in range(4)]
        self.ts(self.uext[:, :, 0:4], self.uext[:, :, 0:4], k, None, ALU.mult, None, UK + ["keep"], UK)
        if full:
            self.cp("act", self.Sbf[:, 0, :, :], self.St[:], SK, [("Sbf", 0, b) for b in range(6)])

    def fused_program(self):
        d = self.dram
        tiles = []
        for t in range(NTF):
            tiles.append(dict(l=0, t=t, full=True, rin="xlay", rout="r1", out_t=t, final=False, mask=t < NPRE, first=(t == 0), own0=False))
        for t in range(NPRE):
            tiles.append(dict(l=1, t=t, full=False, rin="r1", rout=None, out_t=None, final=False, mask=True, first=(t == 0), own0=False))
        for t in range(NPRE, NTF):
            tiles.append(dict(l=1, t=t, full=True, rin="r1", rout="y", out_t=t - NPRE, final=True, mask=False, first=False, own0=(t == NPRE)))

        def xload(i):
            td = tiles[i]
            k = i % 2
            self.dma("sp", self.xtok_b[k][:], d[td["rin"]][td["t"] * TT:(td["t"] + 1) * TT, :].rearrange("(b p) d -> p b d", p=P),
                     [("dram", "res%d" % td["l"], td["t"])], [("xtok", k, b) for b in range(4)])
        xload(0)
        for i, td in enumerate(tiles):
            l = td["l"]
            pv = self.pv[:, l * NPV:(l + 1) * NPV]
            dv = self.dv[:, l, :]
            DVK = [("dv", l, j) for j in range(6)] + ["pv"]
            if td["first"]:
                self.layer_consts(l, True)
                self.zero_states()
            if td["own0"]:
                SK = [("S", b) for b in range(6)]
                self.cp("act", self.Sbf[:, 0, :, :], self.St[:], SK, [("Sbf", 0, b) for b in range(6)])
            if i + 1 < len(tiles):
                xload(i + 1)
            self.xi = i % 2
            self.xtok = self.xtok_b[self.xi]
            self.tile(l, td["t"], td["full"], d[td["rin"]], d[td["rout"]] if td["rout"] else None, pv, dv, DVK,
                      out_t=td["out_t"], final=td["final"])
            if td["mask"]:
                self.mask_states(td["t"], td["full"])

    def halo(self, l):
        d = self.dram
        if l == 0:
            self.dma("sp", self.hal[:], d["halo0"][:, :], (), ["hal"])
        else:
            for j in range(8):
                self.dma("sp", self.halj[:], d["hl_all"][4 * j:4 * j + 4, :], [("dram", "hl_all")], ["halj"])
                if j == 0:
                    self.ts(self.hal[:], self.halj[:], self.hm[0:4, 0:1], None, ALU.mult, None, ["halj", "hm"], ["hal"])
                else:
                    self.stt(self.hal[:], self.halj[:], self.hm[0:4, j:j + 1], self.hal[:], ALU.mult, ALU.add, ["halj", "hm", "hal"], ["hal"])
        b = self.ps.get()
        pf = self.ps_t[b]

        def fn(e):
            inst = None
            for kc in range(8):
                inst = e.transpose(pf[:, kc * 4:(kc + 1) * 4], self.hal[0:4, kc * P:(kc + 1) * P], self.consts[0:4, 0:4])
            return inst
        self.op("pe", fn, ["hal", "consts"], [("ps", b)])
        self.cp("dve", self.xTh[:].rearrange("p k t -> p (k t)"), pf[:, 0:32], [("ps", b)], [("xTh", kc) for kc in range(8)])
        self.ps.put(b)
        self.halo_pending = True

    def write_states(self, l, dv, DVK):
        d = self.dram
        name = f"st{l}"
        st = d[name]
        z = lambda ap, keys: self.op("dve", lambda e: e.memset(ap, 0.0), (), keys)
        z(self.misc[:], ["misc"])
        self.act(self.misc[:, 0:2], self.gsum[:, 0:2], AF.Exp, ["gsum"], ["misc"], scale=-1.0 / 16)
        self.act(self.misc[:, 2:6], self.gsum[:, 2:6], AF.Exp, ["gsum"], ["misc"], scale=1.0)
        self.tt(self.rtmp[:], self.rsum[:], dv[:, 0:4], ALU.mult, ["rsum"] + DVK, ["rtmp"])
        self.act(self.misc[:, 6:10], self.rtmp[:], AF.Exp, ["rtmp"], ["misc"])
        self.cp("dve", self.misc[:, 10:14], self.hc[:], ["hc", "misc"], ["misc"])
        k1 = self.dma("sp", st[0:6 * P, :].rearrange("(b p) c -> p b c", p=P), self.St[:], [("S", b) for b in range(6)], [("dram", name), ("out", name + "a")])
        k2 = self.dma("sp", st[6 * P:7 * P, :], self.misc[:], ["misc"], [("dram", name + "m"), ("out", name + "b")])
        self._outk += [name + "a", name + "b"]

    def combine_states(self, l):
        d = self.dram
        alln = d[f"st{l}_all"]
        for j in range(8):
            self.dma("sp", self.stl[:], alln[j * ST_ROWS:(j + 1) * ST_ROWS, :].rearrange("(b p) c -> p b c", p=P),
                     [("dram", f"st{l}_all")], ["stl"])
            mj = self.mk[:, j:j + 1]
            self.ts(self.deff[:, 0:10], self.stl[:, 6, 0:10], -1.0, mj, ALU.add, ALU.mult, ["stl", "mk"], ["deff"])
            self.ts(self.deff[:, 0:10], self.deff[:, 0:10], 1.0, None, ALU.add, None, ["deff"], ["deff"])
            self.ts(self.stl[:, 0:6, :], self.stl[:, 0:6, :], mj, None, ALU.mult, None, ["stl", "mk"], ["stl"])
            self.ts(self.stl[:, 6, 10:14], self.stl[:, 6, 10:14], mj, None, ALU.mult, None, ["stl", "mk"], ["stl"])
            for b in range(6):
                self.stt(self.St[:, b, :], self.St[:, b, :], self.deff[:, b:b + 1], self.stl[:, b, :], ALU.mult, ALU.add,
                         ["stl", "deff", ("S", b)], [("S", b)])
            self.tt(self.hc[:], self.hc[:], self.deff[:, 6:10], ALU.mult, ["hc", "deff"], ["hc"])
            self.tt(self.hc[:], self.hc[:], self.stl[:, 6, 10:14], ALU.add, ["hc", "stl"], ["hc"])
        self.cp("act", self.Sbf[:, 0, :, :], self.St[:], [("S", b) for b in range(6)], [("Sbf", 0, b) for b in range(6)])

    def allgather(self, p):
        d = self.dram
        nm = {"AG0": ("st0", "st0_all"), "AG1": ("st1", "st1_all"), "AGH": ("hl", "hl_all")}[p]
        src, dst = d[nm[0]], d[nm[1]]
        rk = [("dram", nm[0])] + ([("dram", nm[0] + "m")] if p != "AGH" else [])
        self.op("pool", lambda e: e.collective_compute("AllGather", ALU.bypass, replica_groups=[list(range(8))],
                                                       ins=[src[:, :]], outs=[dst[:, :]]), rk, [("dram", nm[1])], dma=True)

    def tile(self, l, t, full, res_in, res_out, pv, dv, DVK, out_t=None, final=True):
        fused = self.mode == "fused"
        if out_t is None:
            out_t = t
        XK = [("xtok", self.xi, b) for b in range(4)]
        if not fused:
            self.dma("sp", self.xtok[:], res_in[t * TT:(t + 1) * TT, :].rearrange("(b p) d -> p b d", p=P),
                     [("dram", "res%d" % l, t)], XK)
        self.transpose_tok(self.xtok, XK, self.xT, "xT")
        stop = self.dbg.get("stop")
        YK = [("yT", i) for i in range(12)]
        self.rg_branch(l, t, full, pv, dv, DVK)
        if stop == "rg":
            self.dump("dbg_y", self.yT[:, 0:4, :], [P, 4, TT], YK[0:4], BF16)
            return "stop"
        self.attn_branch(l, t, full, "gla", pv, dv, DVK)
        if stop == "gla":
            self.dump("dbg_y", self.yT[:, 0:8, :], [P, 8, TT], YK[0:8], BF16)
            return "stop"
        self.attn_branch(l, t, full, "hg", pv, dv, DVK)
        if not full:
            return
        if stop == "hg":
            self.dump("dbg_y", self.yT[:], [P, 12, TT], YK, BF16)
            return "stop"
        self.merge_out(l, t)
        if stop == "merge":
            self.dump("dbg_x", self.xtok[:], [P, 4, D], XK)
            return "stop"
        self.layernorm(0)
        if stop == "ln1":
            self.dump("dbg_x", self.xtok[:], [P, 4, D], XK)
            return "stop"
        self.transpose_tok(self.xtok, XK, self.x1T, "x1T")
        if l == 0:
            self.ffn_dense()
        else:
            self.moe()
        self.layernorm(2)
        wk = [("dram", "res%d" % (l + 1), t)]
        if final:
            wk.append(("out", "res%d_%d" % (l, t)))
            self._outk.append("res%d_%d" % (l, t))
        self.dma("sp", res_out[out_t * TT:(out_t + 1) * TT, :].rearrange("(b p) d -> p b d", p=P), self.xtok[:], XK, wk)

    def rg_branch(self, l, t, full, pv, dv, DVK):
        sl = self.slab_load(f"win{l}_rg", 8, 512)
        if self.halo_pending:
            for c in range(4):
                b = self.proj_fm(sl, self.xTh, "xTh", c * P, N=4)
                self.cp("dve", self.uext[:, c, 0:4], self.ps_t[b][:, 0:4], [("ps", b)], [("uext", c)])
                self.ps.put(b)
            self.halo_pending = False
        for c in range(4):
            b = self.proj_fm(sl, self.xT, "xT", c * P)
            self.cp("act", self.uext[:, c, 4:4 + TT], self.ps_t[b][:, :], [("ps", b)], [("uext", c)])
            self.ps.put(b)
        self.slabs.put(sl)
        for c in range(4):
            R = self.ring
            ic = R.get()
            cc = self.ring_t[ic]
            ck = ("ring", ic)
            self.ts(cc[:], self.uext[:, c, 4:4 + TT], pv[:, c * 4 + 3:c * 4 + 4], pv[:, 16 + c:17 + c], ALU.mult, ALU.add,
                    [("uext", c), "pv"], [ck])
            for j in range(3):
                self.stt(cc[:], self.uext[:, c, 1 + j:1 + j + TT], pv[:, c * 4 + j:c * 4 + j + 1], cc[:], ALU.mult, ALU.add,
                         [("uext", c), "pv", ck], [ck])
            self.cp("dve", self.uext[:, c, 0:4], self.uext[:, c, TT:TT + 4], [("uext", c)], [("uext", c)])
            ib = R.get()
            ccb = self.ring_t[ib][:].bitcast(BF16)[:, 0:TT]
            self.cp("act", ccb, cc[:], [ck], [("ring", ib)])
            br = self.ps.get()
            self.mm(self.ps_t[br][:, :], self.wbd[:, 0, c, :], ccb, True, True, [("wbd", 0), ("ring", ib)], [("ps", br)])
            bi = self.ps.get()
            self.mm(self.ps_t[bi][:, :], self.wbd[:, 1, c, :], ccb, True, True, [("wbd", 1), ("ring", ib)], [("ps", bi)])
            R.put(ib)
            ir = R.get()
            r = self.ring_t[ir]
            if full:
                self.act(r[:], self.ps_t[br][:, :], AF.Sigmoid, [("ps", br), "pv"], [("ring", ir)], bias=pv[:, 20 + c:21 + c])
            else:
                self.act(r[:], self.ps_t[br][:, :], AF.Sigmoid, [("ps", br), "pv"], [("ring", ir), "rtmp"], bias=pv[:, 20 + c:21 + c],
                         accum_out=self.rtmp[:, c:c + 1])
                self.tt(self.rsum[:, c:c + 1], self.rsum[:, c:c + 1], self.rtmp[:, c:c + 1], ALU.add, ["rtmp", "rsum"], ["rsum"])
            self.ps.put(br)
            ii = R.get()
            gi = self.ring_t[ii]
            self.act(gi[:], self.ps_t[bi][:, :], AF.Sigmoid, [("ps", bi), "pv"], [("ring", ii)], bias=pv[:, 24 + c:25 + c])
            self.ps.put(bi)
            self.tt(gi[:], gi[:], cc[:], ALU.mult, [("ring", ii), ck], [("ring", ii)])
            R.put(ic)
            ia = R.get()
            a = self.ring_t[ia]
            self.act(a[:], r[:], AF.Exp, [("ring", ir)] + DVK, [("ring", ia)], scale=dv[:, c:c + 1])
            self.act(r[:], r[:], AF.Exp, [("ring", ir)] + DVK, [("ring", ir)], scale=dv[:, 4 + c:5 + c])
            self.act(r[:], r[:], AF.Sqrt, [("ring", ir)], [("ring", ir)], scale=-1.0, bias=1.0)
            self.tt(gi[:], gi[:], r[:], ALU.mult, [("ring", ii), ("ring", ir)], [("ring", ii)])
            self.op("dve", lambda e, o=r[:], a_=a[:], b_=gi[:], h0=self.hc[:, c:c + 1]: e.tensor_tensor_scan(
                out=o, data0=a_, data1=b_, initial=h0, op0=ALU.mult, op1=ALU.add),
                [("ring", ia), ("ring", ii), "hc"], [("ring", ir)])
            self.cp("dve", self.hc[:, c:c + 1], r[:, TT - 1:TT], [("ring", ir), "hc"], ["hc"])
            R.put(ia)
            R.put(ii)
            if full:
                self.cp("pool", self.yT[:, c, :], r[:], [("ring", ir)], [("yT", c)])
            R.put(ir)

    def _dep_of(self, key):
        w = self.S.lastw.get(key)
        return {w} if w is not None else set()

    def attn_branch(self, l, t, full, kind, pv, dv, DVK):
        R = self.ring
        gla = kind == "gla"
        nb = 2 if gla else 4
        sb0 = 0 if gla else 2
        sgn = (-1.0 / 16) if gla else 1.0
        qscale = (64 ** -0.5) if gla else (128 ** -0.5)
        Kh = 64 if gla else 128
        heads = [(h // 2, (h % 2) * 64) for h in range(4)] if gla else [(h, 0) for h in range(4)]
        sl = self.slab_load(f"win{l}_" + ("gv" if gla else "hi"), 8, 512)
        for blk in range(4):
            b = self.proj_tm(sl, self.xT, "xT", blk)
            self.cp("act" if blk % 2 else "dve", self.vtok[:, blk, :], self.ps_t[b][:, :], [("ps", b)], [("vtok", blk)])
            self.ps.put(b)
        self.slabs.put(sl)
        if gla:
            sla = self.slab_load(f"win{l}_ga", 8, 16)
            b = self.proj_fm(sla, self.xT, "xT", 0, M=16)
            self.cp("act", self.alow[:], self.ps_t[b][0:16, :], [("ps", b)], ["alow"])
            self.ps.put(b)
            self.slabs.put(sla)
            slq = self.slab_load(f"win{l}_qk", 8, 512)
        else:
            slf = self.slab_load(f"win{l}_hf", 8, 512)
            slq = self.slab_load(f"win{l}_hq", 8, 512) if full else None
        for fb in range(nb):
            ig = R.get()
            G = self.ring_t[ig]
            gk = ("ring", ig)
            ikk = None
            if gla:
                b = self.ps.get()
                self.mm(self.ps_t[b][:, :], self.wa[0:16, fb * P:(fb + 1) * P], self.alow[0:16, :], True, True, ["wa", "alow"], [("ps", b)])
                self.act(G[:], self.ps_t[b][:, :], AF.Exp, [("ps", b)] + DVK, [gk], scale=-1.0, bias=dv[:, 8 + fb:9 + fb])
                self.ps.put(b)
                self.act(G[:], G[:], AF.Ln, [gk], [gk], bias=1.0)
            else:
                b = self.proj_fm(slf, self.xT, "xT", fb * P)
                ikk = R.get()
                kk = self.ring_t[ikk]
                self.act(kk[:], self.ps_t[b][:, :], AF.Sigmoid, [("ps", b)], [("ring", ikk)])
                self.ps.put(b)
                self.ts(G[:], kk[:], dv[:, 14 + fb:15 + fb], dv[:, 10 + fb:11 + fb], ALU.mult, ALU.add, [("ring", ikk)] + DVK, [gk])
                self.act(G[:], G[:], AF.Ln, [gk], [gk])
                self.ts(kk[:], kk[:], dv[:, 18 + fb:19 + fb], dv[:, 14 + fb:15 + fb], ALU.mult, ALU.add, [("ring", ikk)] + DVK, [("ring", ikk)])
            ie = R.get()
            Gc = self.ring_t[ie]
            self.op("dve", lambda e, o=Gc[:], i_=G[:], m=self.consts[:, 128:640]: e.tensor_tensor_scan(
                out=o, data0=m, data1=i_, initial=0.0, op0=ALU.mult, op1=ALU.add), [gk, "consts"], [("ring", ie)])
            ig, ie = ie, ig
            G, gk = Gc, ("ring", ig)
            G3 = G[:].rearrange("p (c s) -> p c s", s=64)
            sb = sb0 + fb
            self.act(self.dch[:, sb, :], G3[:, :, 63], AF.Exp, [gk], [("dch", sb)], scale=sgn)
            if not full:
                self.op("dve", lambda e, o=self.gtmp[:, sb:sb + 1], i_=G3[:, :, 63]: e.tensor_reduce(out=o, in_=i_, axis=AX.X, op=ALU.add),
                        [gk], ["gtmp"])
                self.tt(self.gsum[:, sb:sb + 1], self.gsum[:, sb:sb + 1], self.gtmp[:, sb:sb + 1], ALU.add, ["gtmp", "gsum"], ["gsum"])
            E = self.ring_t[ie]
            ek = ("ring", ie)
            E3 = E[:].rearrange("p (c s) -> p c s", s=64)
            self.tt(E3, G3[:, :, 63:64].broadcast_to([P, 8, 64]), G3, ALU.subtract, [gk], [ek])
            self.act(E[:], E[:], AF.Exp, [ek], [ek], scale=sgn)
            if gla:
                bk = self.proj_fm(slq, self.xT, "xT", 256 + fb * P)
                self.tt(self.kd[:, fb, :], self.ps_t[bk][:, :], E[:], ALU.mult, [("ps", bk), ek], [("kd", fb)])
            else:
                self.tt(self.kd[:, fb, :], kk[:], E[:], ALU.mult, [("ring", ikk), ek], [("kd", fb)])
            if full:
                self.act(E[:], G[:], AF.Exp, [gk, ek], [ek], scale=-sgn)
                if gla:
                    self.tt(self.kt[:, fb, :], self.ps_t[bk][:, :], E[:], ALU.mult, [("ps", bk), ek], [("kt", fb)])
                else:
                    self.tt(self.kt[:, fb, :], kk[:], E[:], ALU.mult, [("ring", ikk), ek], [("kt", fb)])
                self.act(E[:], G[:], AF.Exp, [gk, ek], [ek], scale=sgn)
                if gla:
                    bq = self.proj_fm(slq, self.xT, "xT", fb * P)
                    for hh in range(2):
                        qi = 2 * hh + fb
                        oth = slice((1 - hh) * 64, (2 - hh) * 64)
                        own = slice(hh * 64, (hh + 1) * 64)
                        self.op("pool", lambda e, o=self.qt[oth, qi, :]: e.memset(o, 0.0), (), [("qt", qi)])
                        self.stt(self.qt[own, qi, :], self.ps_t[bq][own, :], qscale, E[own, :], ALU.mult, ALU.mult,
                                 [("ps", bq), ek, ("qt", qi)], [("qt", qi)])
                    self.ps.put(bq)
                else:
                    bq = self.proj_fm(slq, self.xT, "xT", fb * P)
                    self.act(G[:], self.ps_t[bq][:, :], AF.Silu, [("ps", bq), gk], [gk])
                    self.ps.put(bq)
                    self.stt(self.qt[:, fb, :], G[:], qscale, E[:], ALU.mult, ALU.mult, [gk, ek], [("qt", fb)])
            if gla:
                self.ps.put(bk)
            else:
                R.put(ikk)
            R.put(ie)
            R.put(ig)
            b = self.ps.get()
            pbf = self.ps_t[b][:].bitcast(BF16)

            def fn(e, fb=fb, pbf=pbf):
                inst = None
                for blk in range(4):
                    inst = e.transpose(pbf[:, blk * P:(blk + 1) * P], self.kd[:, fb, blk * P:(blk + 1) * P], self.ident_bf[:])
                return inst
            self.op("pe", fn, [("kd", fb), "ident"], [("ps", b)])
            self.cp("act", self.kdtok[:, fb, :, :].rearrange("p b f -> p (b f)"), pbf[:, 0:512], [("ps", b)], [("kdtok", fb)])
            self.ps.put(b)
        self.slabs.put(slq) if slq is not None else None
        if not gla:
            self.slabs.put(slf)
        po = None
        if full:
            po = [self.ps.get() for _ in range(4)]
        for c in range(0, 8, 2):
            blk = c // 2
            if full and c % 2 == 0:
                pa = self.ps.get()
                for cc_ in (c, c + 1):
                    hp_ = (cc_ % 2) * 64
                    for h, (fb, kp) in enumerate(heads):
                        qi = (2 * (kp // 64) + fb) if gla else fb
                        self.mm(self.ps_t[pa][hp_:hp_ + 64, h * 64:(h + 1) * 64],
                                self.kt[:, fb, cc_ * 64:(cc_ + 1) * 64], self.qt[:, qi, cc_ * 64:(cc_ + 1) * 64],
                                True, True, [("kt", fb), ("qt", qi)], [("ps", pa)])
                for pz in range(2):
                    self.tt(self.Asb[:, pz, blk, :], self.ps_t[pa][:, 0:256], self.causal[:, pz, :], ALU.mult, [("ps", pa), "causal"], [("Asb", pz, blk)])
                self.ps.put(pa)
        for c in range(8):
            blk, hp = c // 2, (c % 2) * 64
            par = c % 2
            pu = self.ps.get()
            for h, (fb, kp) in enumerate(heads):
                if gla:
                    out = self.ps_t[pu][kp:kp + 64, fb * P:(fb + 1) * P]
                    lhs = self.kdtok[hp:hp + 64, fb, blk, kp:kp + 64]
                else:
                    out = self.ps_t[pu][:, fb * P:(fb + 1) * P]
                    lhs = self.kdtok[hp:hp + 64, fb, blk, :]
                self.mm(out, lhs, self.vtok[hp:hp + 64, blk, h * P:(h + 1) * P], True, True, [("kdtok", fb), ("vtok", blk)], [("ps", pu)])
            for fb in range(nb):
                sbk = sb0 + fb
                self.stt(self.St[:, sbk, :], self.St[:, sbk, :], self.dch[:, sbk, c:c + 1], self.ps_t[pu][:, fb * P:(fb + 1) * P], ALU.mult, ALU.add,
                         [("S", sbk), ("dch", sbk), ("ps", pu)], [("S", sbk)])
            self.ps.put(pu)
            if full:
                self.cp("act", self.Sbf[:, 1 - par, sb0:sb0 + nb, :], self.St[:, sb0:sb0 + nb, :], [("S", sb0 + i) for i in range(nb)],
                        [("Sbf", 1 - par, sb0 + i) for i in range(nb)])
            if full:
                for h, (fb, kp) in enumerate(heads):
                    sbk = sb0 + fb
                    qi = (2 * (kp // 64) + fb) if gla else fb
                    out = self.ps_t[po[h]][:, c * 64:(c + 1) * 64]
                    self.mm(out, self.vtok[:, blk, h * P:(h + 1) * P], self.Asb[:, par, blk, h * 64:(h + 1) * 64], True, False,
                            [("vtok", blk), ("Asb", par, blk)], [("ps", po[h])])
                    self.mm(out, self.Sbf[:, par, sbk, :], self.qt[:, qi, c * 64:(c + 1) * 64], False, True,
                            [("Sbf", par, sbk), ("qt", qi)], [("ps", po[h])])
        if not full:
            return
        slg = self.slab_load(f"win{l}_" + ("gg" if gla else "hg"), 8, 512)
        nw0 = 34 if gla else 43
        sq = []
        if not gla:
            pn = self.ps.get()
        for h in range(4):
            isq = R.get()
            sqb = self.ring_t[isq][:].bitcast(BF16)[:, 0:TT]
            self.act(sqb, self.ps_t[po[h]][:, :], AF.Square, [("ps", po[h])], [("ring", isq)])
            if gla:
                pn = self.ps.get()
                self.mm(self.ps_t[pn][:, :], self.ones_g[:], sqb, True, True, ["ones_g", ("ring", isq)], [("ps", pn)])
                R.put(isq)
                irs = R.get()
                rs = self.ring_t[irs]
                self.act(rs[:], self.ps_t[pn][:, :], AF.Ln, [("ps", pn)], [("ring", irs)], bias=RMS_EPS)
                self.ps.put(pn)
                self.act(rs[:], rs[:], AF.Exp, [("ring", irs)], [("ring", irs)], scale=-0.5)
                self.gate_out(l, h, slg, po[h], rs, irs, pv[:, nw0:nw0 + 1], AF.Silu, 4 + h)
                R.put(irs)
            else:
                self.mm(self.ps_t[pn][:, :], self.ones_h[:], sqb, h == 0, h == 3, ["ones_h", ("ring", isq)], [("ps", pn)])
                sq.append(isq)
        if not gla:
            for isq in sq:
                R.put(isq)
            irs = R.get()
            rs = self.ring_t[irs]
            self.act(rs[:], self.ps_t[pn][:, :], AF.Ln, [("ps", pn)], [("ring", irs)], bias=RMS_EPS)
            self.ps.put(pn)
            self.act(rs[:], rs[:], AF.Exp, [("ring", irs)], [("ring", irs)], scale=-0.5)
            for h in range(4):
                self.gate_out(l, h, slg, po[h], rs, irs, pv[:, nw0 + h:nw0 + h + 1], AF.Sigmoid, 8 + h)
            R.put(irs)
        for h in range(4):
            self.ps.put(po[h])
        self.slabs.put(slg)

    def gate_out(self, l, h, slg, pob, rs, irs, nw, func, yc):
        R = self.ring
        bg = self.proj_fm(slg, self.xT, "xT", h * P)
        isg = R.get()
        sg = self.ring_t[isg]
        self.act(sg[:], self.ps_t[bg][:, :], func, [("ps", bg)], [("ring", isg)])
        self.ps.put(bg)
        self.tt(sg[:], sg[:], rs[:], ALU.mult, [("ring", isg), ("ring", irs)], [("ring", isg)])
        self.stt(self.yT[:, yc, :], self.ps_t[pob][:, :], nw, sg[:], ALU.mult, ALU.mult, [("ps", pob), "pv", ("ring", isg)], [("yT", yc)])
        R.put(isg)

    def merge_out(self, l, t):
        R = self.ring
        for dh in range(2):
            slb = [self.slab_load(f"wbr{l}_{n}_{dh}", 4, 512) for n in range(3)]
            accs = []
            for dcl in range(4):
                ia = R.get()
                accs.append(ia)
            for n in range(3):
                pbs = []
                for dcl in range(4):
                    pb = self.ps.get()
                    st = self.slab_t[slb[n]]
                    pairs = [(st[:, kc, dcl * P:(dcl + 1) * P], self.yT[:, n * 4 + kc, :]) for kc in range(4)]
                    self.mmk(self.ps_t[pb][:, :], pairs, [("slab", slb[n])] + [("yT", n * 4 + kc) for kc in range(4)], [("ps", pb)])
                    pbs.append(pb)
                self.slabs.put(slb[n])
                slgt = self.slab_load(f"win{l}_gt{n * 2 + dh}", 8, 512)
                for dcl in range(4):
                    pg = self.proj_fm(slgt, self.xT, "xT", dcl * P)
                    isg = R.get()
                    sg = self.ring_t[isg]
                    self.act(sg[:], self.ps_t[pg][:, :], AF.Sigmoid, [("ps", pg)], [("ring", isg)])
                    self.ps.put(pg)
                    acc = self.ring_t[accs[dcl]]
                    ak = ("ring", accs[dcl])
                    if n == 0:
                        self.tt(acc[:], self.ps_t[pbs[dcl]][:, :], sg[:], ALU.mult, [("ps", pbs[dcl]), ("ring", isg)], [ak])
                    else:
                        self.tt(sg[:], self.ps_t[pbs[dcl]][:, :], sg[:], ALU.mult, [("ps", pbs[dcl]), ("ring", isg)], [("ring", isg)])
                        if n == 1:
                            self.tt(acc[:], acc[:], sg[:], ALU.add, [ak, ("ring", isg)], [ak])
                        else:
                            self.tt(self.mixT[:, dh * 4 + dcl, :], acc[:], sg[:], ALU.add, [ak, ("ring", isg)], [("mixT", dh * 4 + dcl)])
                    self.ps.put(pbs[dcl])
                    R.put(isg)
                self.slabs.put(slgt)
            for ia in accs:
                R.put(ia)
        for dh in range(2):
            slo = self.slab_load(f"wout{l}_{dh}", 8, 512)
            for blk in range(4):
                b = self.proj_tm(slo, self.mixT, "mixT", blk)
                xs = self.xtok[:, blk, dh * 512:(dh + 1) * 512]
                self.stt(xs, xs, ALPHA, self.ps_t[b][:, :], ALU.mult, ALU.add, [("ps", b), ("xtok", self.xi, blk)], [("xtok", self.xi, blk)])
                self.ps.put(b)
            self.slabs.put(slo)

    def layernorm(self, j0):
        for blk in range(4):
            xk = ("xtok", self.xi, blk)
            for hf in range(2):
                self.op("dve", lambda e, o=self.lnst[:, blk, hf, :], i_=self.xtok[:, blk, hf * 512:(hf + 1) * 512]: e.bn_stats(out=o, in_=i_),
                        [xk], [("lnst", blk)])
            self.op("dve", lambda e, o=self.lnmv[:, blk, :], i_=self.lnst[:, blk, :, :].rearrange("p a b -> p (a b)"): e.bn_aggr(out=o, in_=i_),
                    [("lnst", blk)], [("lnmv", blk)])
            self.act(self.lnr[:, blk, 0:1], self.lnmv[:, blk, 1:2], AF.Sqrt, [("lnmv", blk)], [("lnr", blk)], bias=LN_EPS)
            self.op("dve", lambda e, o=self.lnr[:, blk, 1:2], i_=self.lnr[:, blk, 0:1]: e.reciprocal(out=o, in_=i_), [("lnr", blk)], [("lnr", blk)])
            xs = self.xtok[:, blk, :]
            self.ts(xs, xs, self.lnmv[:, blk, 0:1], self.lnr[:, blk, 1:2], ALU.subtract, ALU.mult, [xk, ("lnmv", blk), ("lnr", blk)], [xk])
            self.tt(xs, xs, self.lnp[:, j0, :], ALU.mult, [xk, ("lnp", j0)], [xk], eng="pool")
            self.tt(xs, xs, self.lnp[:, j0 + 1, :], ALU.add, [xk, ("lnp", j0 + 1)], [xk], eng="pool")

    def up_proj(self, names_g, names_u, ngroups, nfc_of):
        R = self.ring
        fc = 0
        for g in range(ngroups):
            nf = nfc_of(g)
            sg_ = self.slab_load(names_g(g), 8, nf * P)
            su_ = self.slab_load(names_u(g), 8, nf * P)
            for j in range(nf):
                pg = self.proj_fm(sg_, self.x1T, "x1T", j * P)
                pu = self.proj_fm(su_, self.x1T, "x1T", j * P)
                isg = R.get()
                sg = self.ring_t[isg]
                self.act(sg[:], self.ps_t[pg][:, :], AF.Silu, [("ps", pg)], [("ring", isg)])
                self.ps.put(pg)
                self.tt(self.hT[:, fc, :], self.ps_t[pu][:, :], sg[:], ALU.mult, [("ps", pu), ("ring", isg)], [("hT", fc)])
                self.ps.put(pu)
                R.put(isg)
                fc += 1
            self.slabs.put(sg_)
            self.slabs.put(su_)
        return fc

    def down_proj(self, names_d, ngroups, nfc_of, evac):
        for dh in range(2):
            pbs = [self.ps.get() for _ in range(4)]
            fc = 0
            nfc_tot = sum(nfc_of(g) for g in range(ngroups))
            for g in range(ngroups):
                nf = nfc_of(g)
                sd = self.slab_load(names_d(g, dh), nf, 512)
                st = self.slab_t[sd]
                for j in range(nf):
                    for blk in range(4):
                        self.mm(self.ps_t[pbs[blk]][:, :], self.hT[:, fc, blk * P:(blk + 1) * P], st[:, j, :], fc == 0, fc == nfc_tot - 1,
                                [("hT", fc), ("slab", sd)], [("ps", pbs[blk])])
                    fc += 1
                self.slabs.put(sd)
            for blk in range(4):
                evac(blk, dh, pbs[blk])
                self.ps.put(pbs[blk])

    def ffn_dense(self):
        nfc = lambda g: 4 if g < 5 else 2
        self.up_proj(lambda g: f"fg_{g}", lambda g: f"fu_{g}", 6, nfc)

        def evac(blk, dh, pb):
            xs = self.xtok[:, blk, dh * 512:(dh + 1) * 512]
            self.stt(xs, xs, ALPHA, self.ps_t[pb][:, :], ALU.mult, ALU.add, [("ps", pb), ("xtok", self.xi, blk)], [("xtok", self.xi, blk)])
        self.down_proj(lambda g, dh: f"fd_{g}_{dh}", 6, nfc, evac)

    def moe(self):
        R = self.ring
        for blk in range(4):
            irt = [R.get(), R.get()]
            rtf = [self.ring_t[i][:].rearrange("p (k t) -> p k t", t=P) for i in irt]
            for half in range(2):
                b = self.ps.get()

                def fn(e, blk=blk, half=half, b=b, xt=self.xtok):
                    inst = None
                    for k4 in range(4):
                        kc = half * 4 + k4
                        inst = e.transpose(self.ps_t[b][:, k4 * P:(k4 + 1) * P], xt[:, blk, kc * P:(kc + 1) * P], self.consts[:, 0:128])
                    return inst
                self.op("pe", fn, [("xtok", self.xi, blk), "consts"], [("ps", b)])
                self.cp("act" if half else "dve", self.ring_t[irt[half]][:], self.ps_t[b][:, :], [("ps", b)], [("ring", irt[half])])
                self.ps.put(b)
            b = self.ps.get()
            pairs = [(rtf[kc // 4][:, kc % 4, :], self.rw[:, kc, :]) for kc in range(8)]
            self.mmk(self.ps_t[b][:, 0:8], pairs, [("ring", irt[0]), ("ring", irt[1]), "rw"], [("ps", b)])
            R.put(irt[0])
            R.put(irt[1])
            rl = self.rl[:, blk, :]
            rk = ("rl", blk)
            self.cp("dve", rl, self.ps_t[b][:, 0:8], [("ps", b)], [rk])
            self.ps.put(b)
            m8 = self.rm8[:, blk, :]
            self.op("dve", lambda e, o=m8, i_=rl: e.max(out=o, in_=i_), [rk], [("rm8", blk)])
            sm = self.rsm[:, blk, :]
            self.ts(sm[:, 0:1], m8[:, 0:1], -1.0, None, ALU.mult, None, [("rm8", blk)], [("rsm", blk)])
            wt = self.rwt[:, blk, :]
            self.act(wt, rl, AF.Exp, [rk, ("rsm", blk)], [("rwt", blk)], bias=sm[:, 0:1])
            self.ts(rl, rl, m8[:, 1:2], None, ALU.is_ge, None, [rk, ("rm8", blk)], [rk])
            self.tt(wt, wt, rl, ALU.mult, [("rwt", blk), rk], [("rwt", blk)])
            self.op("dve", lambda e, o=sm[:, 1:2], i_=wt: e.tensor_reduce(out=o, in_=i_, axis=AX.X, op=ALU.add), [("rwt", blk)], [("rsm", blk)])
            self.op("dve", lambda e, o=sm[:, 2:3], i_=sm[:, 1:2]: e.reciprocal(out=o, in_=i_), [("rsm", blk)], [("rsm", blk)])
            self.ts(wt, wt, sm[:, 2:3], None, ALU.mult, None, [("rwt", blk), ("rsm", blk)], [("rwt", blk)])
            xs = self.xtok[:, blk, :]
            self.ts(xs, xs, ALPHA, None, ALU.mult, None, [("xtok", self.xi, blk)], [("xtok", self.xi, blk)])
        nfc = lambda g: 4
        for ex in range(8):
            self.up_proj(lambda g: f"mg_{ex}_{g}", lambda g: f"mu_{ex}_{g}", 7, nfc)

            def evac(blk, dh, pb, ex=ex):
                xs = self.xtok[:, blk, dh * 512:(dh + 1) * 512]
                self.stt(xs, self.ps_t[pb][:, :], self.rwt[:, blk, ex:ex + 1], xs, ALU.mult, ALU.add,
                         [("ps", pb), ("xtok", self.xi, blk), ("rwt", blk)], [("xtok", self.xi, blk)])
            self.down_proj(lambda g, dh: f"md_{ex}_{g}_{dh}", 7, nfc, evac)


def _consts():
    c = np.zeros((P, 1024), np.float32)
    c[:, 0:128] = np.eye(P, dtype=np.float32)
    m = np.ones((P, 512), np.float32)
    m[:, 0::64] = 0.0
    c[:, 128:640] = m
    s = np.arange(P) % 64
    tcol = np.arange(256) % 64
    c[:, 640:896] = (s[:, None] <= tcol[None, :]).astype(np.float32)
    c[:, 896:1024] = 1.0
    return c


def _pvec(inp):
    def fm(v, nch):
        return np.ascontiguousarray(np.asarray(v, np.float32).reshape(nch, P).T)
    out = np.zeros((P, 2 * NPV), np.float32)
    for l in range(2):
        o = l * NPV
        cw = np.asarray(inp["conv_w"][l], np.float32)
        out[:, o:o + 16] = cw.reshape(4, 4, P).transpose(2, 1, 0).reshape(P, 16)
        out[:, o + 16:o + 20] = fm(inp["conv_b"][l], 4)
        out[:, o + 20:o + 24] = fm(inp["rg_br"][l], 4)
        out[:, o + 24:o + 28] = fm(inp["rg_bi"][l], 4)
        out[:, o + 28:o + 32] = fm(inp["rg_lambda"][l], 4)
        out[:, o + 32:o + 34] = fm(inp["gla_ba"][l], 2)
        out[:, o + 34:o + 35] = fm(inp["gla_norm_w"][l], 1)
        out[:, o + 35:o + 39] = fm(inp["hg_lb_logits"][0], 4)
        out[:, o + 39:o + 43] = fm(inp["hg_lb_logits"][1], 4)
        out[:, o + 43:o + 47] = fm(inp["hg_norm_w"][l], 4)
    return out


def _wbd(inp):
    w = np.zeros((2, 2, P, 4, P), np.float32)
    for l in range(2):
        for j, nm in enumerate(("rg_wr", "rg_wi")):
            a = np.asarray(inp[nm][l], np.float32)
            for g in range(8):
                c, h = g // 2, g % 2
                w[l, j, h * 64:(h + 1) * 64, c, h * 64:(h + 1) * 64] = a[g]
    return w


_PROGS = {}


DEBUG = None


def _prog(phases, mode):
    key = (tuple(phases), mode, str(DEBUG))
    if key not in _PROGS:
        _PROGS[key] = Builder(list(phases), mode, DEBUG).build()
    return _PROGS[key]


WNAMES = ("w_in", "gla_wa", "w_branch", "w_out", "ln1_g", "ln1_b", "ln2_g", "ln2_b", "ffn_wg", "ffn_wu", "ffn_wd",
          "router_w", "moe_wg", "moe_wu", "moe_wd")


def _base_maps(inp):
    x = np.ascontiguousarray(np.asarray(inp["x"], np.float32))
    consts = _consts()
    pvec = _pvec(inp)
    wbd = _wbd(inp)
    shared = {k: np.ascontiguousarray(np.asarray(inp[k], np.float32)) for k in WNAMES}
    maps = []
    for c in range(8):
        b, s = c // 4, c % 4
        xs = x[b, s * SEG:(s + 1) * SEG]
        halo = x[b, s * SEG - 4:s * SEG] if s > 0 else np.zeros((4, D), np.float32)
        mk = np.zeros((P, 8), np.float32)
        hm = np.zeros((P, 8), np.float32)
        for j in range(8):
            if j // 4 == b and j < c:
                mk[:, j] = 1.0
        if s > 0:
            hm[:, c - 1] = 1.0
        m = dict(shared)
        m.update({"x": np.ascontiguousarray(xs), "halo0": np.ascontiguousarray(halo), "masks": mk, "hmask": hm,
                  "consts": consts, "pvec": pvec, "wbd": wbd})
        maps.append(m)
    return maps


def _fused_maps(inp):
    x = np.asarray(inp["x"], np.float32)
    consts = _consts()
    pvec = _pvec(inp)
    wbd = _wbd(inp)
    shared = {k: np.ascontiguousarray(np.asarray(inp[k], np.float32)) for k in WNAMES}
    maps = []
    for c in range(8):
        b, s = c // 4, c % 4
        nreal = (s + 1) * SEG
        xl = np.zeros((NTF * TT, D), np.float32)
        xl[NTF * TT - nreal:] = x[b, 0:nreal]
        keep = np.zeros((P, NTF), np.float32)
        keep[:, (NTF * TT - nreal) // TT:] = 1.0
        m = dict(shared)
        m.update({"xlay": xl, "keep": keep, "consts": consts, "pvec": pvec, "wbd": wbd})
        maps.append(m)
    return maps


BIG = ("w_in", "w_branch", "w_out")
NEED = {"L0P1": ("w_in", "gla_wa", "router_w"),
        "L0P2": ("w_in", "gla_wa", "router_w", "w_branch", "w_out", "ln1_g", "ln1_b", "ln2_g", "ln2_b", "ffn_wg", "ffn_wu", "ffn_wd"),
        "L1P1": ("w_in", "gla_wa", "router_w"),
        "L1P2": ("w_in", "gla_wa", "router_w", "w_branch", "w_out", "ln1_g", "ln1_b", "ln2_g", "ln2_b", "moe_wg", "moe_wu", "moe_wd")}
COMMON = ("x", "halo0", "masks", "hmask", "consts", "pvec", "wbd")


def _run(phases, mode, maps, extra=()):
    nc = _prog(phases, mode)
    if mode == "fused":
        ms = maps
    else:
        p = phases[0]
        l = 0 if p.startswith("L0") else 1
        ms = []
        for m in maps:
            mm = {k: m[k] for k in COMMON if not (l == 1 and k in ("x", "halo0"))}
            for k in NEED[p]:
                mm[k] = np.ascontiguousarray(m[k][l:l + 1]) if k in BIG else m[k]
            for k in extra:
                mm[k] = m[k]
            ms.append(mm)
    res = run_bass_kernel_spmd(nc, ms, core_ids=list(range(8)))
    return res.results


MODE = "fused"


def kernel(**inp):
    if MODE == "fused":
        res = _run(["F"], "fused", _fused_maps(inp))
        y = np.stack([r["y"] for r in res]).reshape(2, 4 * SEG, D)
        return np.ascontiguousarray(y.astype(np.float32))
    maps = _base_maps(inp)
    rA = _run(["L0P1"], "multi", maps)
    st0_all = np.concatenate([r["st0"] for r in rA], axis=0)
    mB = [dict(m, st0_all=st0_all) for m in maps]
    rB = _run(["L0P2"], "multi", mB, extra=("st0_all",))
    r1 = [r["r1"] for r in rB]
    hl_all = np.concatenate([r[-4:] for r in r1], axis=0)
    mC = [dict(m, r1=r1[c], hl_all=hl_all) for c, m in enumerate(maps)]
    rC = _run(["L1P1"], "multi", mC, extra=("r1", "hl_all"))
    st1_all = np.concatenate([r["st1"] for r in rC], axis=0)
    mD = [dict(m, st1_all=st1_all) for m in mC]
    rD = _run(["L1P2"], "multi", mD, extra=("r1", "hl_all", "st1_all"))
    y = np.stack([r["y"] for r in rD]).reshape(2, 4 * SEG, D)
    return np.ascontiguousarray(y.astype(np.float32))
```

```python
import numpy as np
from contextlib import ExitStack
import concourse.bass as bass
import concourse.mybir as mybir
from concourse.bass_utils import run_bass_kernel_spmd

F32 = mybir.dt.float32
BF16 = mybir.dt.bfloat16
AF = mybir.ActivationFunctionType
ALU = mybir.AluOpType
AX = mybir.AxisListType

P = 128
TT = 512
NT = 8
SEG = 4096
D = 1024
DIN = 7184
ALPHA = 4.0 ** 0.25
LN_EPS = 1e-5
RMS_EPS = 1e-6
NPV = 48
C_RG, C_GQ, C_GK, C_GV, C_GA, C_GG, C_HQ, C_HF, C_HI, C_HG, C_GATES = 0, 512, 768, 1024, 1536, 1552, 2064, 2576, 3088, 3600, 4112
ST_ROWS = 7 * P
NTF = 32
NPRE = 24

SAME_ENG_SYNC = True
BULK_FS = 1 << 30
N_SP_SEMS = 24
N_POOL_SEMS = 12


class Sched:
    def __init__(self):
        self.ops = []
        self.lastw = {}
        self.rd = {}

    def add(self, eng, fn, reads=(), writes=(), dma=False, fs=0):
        i = len(self.ops)
        deps = set()
        for k in reads:
            w = self.lastw.get(k)
            if w is not None:
                deps.add(w)
        for k in writes:
            w = self.lastw.get(k)
            if w is not None:
                deps.add(w)
            r = self.rd.get(k)
            if r:
                deps.update(r[0].values())
                deps.update(r[1])
        self.ops.append([eng, fn, deps, dma, bool(dma), None, fs])
        for k in reads:
            r = self.rd.setdefault(k, ({}, []))
            if dma:
                r[1].append(i)
            else:
                r[0][eng] = i
        for k in writes:
            self.lastw[k] = i
            self.rd[k] = ({}, [])
        return i

    def finalize(self, sems):
        ops = self.ops
        for i, op in enumerate(ops):
            for d in op[2]:
                dop = ops[d]
                if (not dop[3]) and dop[0] == op[0] and (not op[3]):
                    if dop[0] == "pe" or not SAME_ENG_SYNC or (dop[6] >= BULK_FS and dop[0] in ("act", "dve")):
                        continue
                dop[4] = True
        cnt = {e: 0 for e in ("pe", "act", "dve", "pool")}
        rr = {"sp": 0, "pool": 0}
        semcum = {}
        semlast = {}
        for i, op in enumerate(ops):
            if not op[4]:
                continue
            if op[3]:
                q = op[0]
                pool = sems["dma_" + q]
                s = pool[rr[q] % len(pool)]
                rr[q] += 1
                prev = semlast.get(id(s))
                if prev is not None:
                    op[2].add(prev)
                semcum[id(s)] = semcum.get(id(s), 0) + 16
                semlast[id(s)] = i
                op[5] = (s, semcum[id(s)], 16)
            else:
                cnt[op[0]] += 1
                op[5] = (sems[op[0]], cnt[op[0]], 1)

    def emit(self, engname, eng):
        ops = self.ops
        waited = {}
        for i, op in enumerate(ops):
            if op[0] != engname:
                continue
            need = {}
            for d in op[2]:
                dop = ops[d]
                sg = dop[5]
                if sg is None:
                    continue
                if (not dop[3]) and dop[0] == engname and (not op[3]):
                    if engname == "pe" or not SAME_ENG_SYNC or (dop[6] >= BULK_FS and engname in ("act", "dve")):
                        continue
                k = id(sg[0])
                if waited.get(k, 0) >= sg[1]:
                    continue
                if k not in need or need[k][1] < sg[1]:
                    need[k] = sg
            for k, sg in need.items():
                eng.wait_ge(sg[0], sg[1])
                waited[k] = sg[1]
            if op[1] is None:
                continue
            inst = op[1](eng)
            if op[5] is not None:
                inst.then_inc(op[5][0], op[5][2])


class Ring:
    def __init__(self, items):
        self.items = items
        self.free = list(range(len(items)))

    def get(self):
        assert self.free, "ring exhausted"
        return self.free.pop(0)

    def put(self, i):
        assert i not in self.free
        self.free.append(i)


class Builder:
    def __init__(self, phases, mode, debug=None):
        self.phases = phases
        self.mode = mode
        self.dbg = debug or {}
        self.nc = bass.Bass("TRN2", target_bir_lowering=False)
        self.S = Sched()
        self.es = ExitStack()
        self.dram = {}
        self.conv_done = set()
        self.outputs = []
        self.out_dmas = []

    def din(self, name, shape, dt=F32):
        t = self.nc.dram_tensor(name, list(shape), dt, kind="ExternalInput").ap()
        self.dram[name] = t
        return t

    def dout(self, name, shape, dt=F32):
        t = self.nc.dram_tensor(name, list(shape), dt, kind="ExternalOutput").ap()
        self.dram[name] = t
        self.outputs.append(name)
        return t

    def dint(self, name, shape, dt=F32):
        t = self.nc.dram_tensor(name, list(shape), dt, kind="Internal").ap()
        self.dram[name] = t
        return t

    def sb(self, name, shape, dt=F32):
        return self.es.enter_context(self.nc.sbuf_tensor(name, list(shape), dt))

    def dump(self, name, ap, shape, keys, dt=F32):
        t = self.dout(name, shape, dt)
        idx = tuple(slice(None) for _ in shape)
        self.dma("sp", t[idx], ap, keys, [("out", name)])
        self._outk.append(name)

    def op(self, eng, fn, r=(), w=(), dma=False, fs=0):
        return self.S.add(eng, fn, r, w, dma, fs)

    @staticmethod
    def fsz(ap):
        n = 1
        for d_ in ap.shape[1:]:
            n *= int(d_)
        return n

    def dma(self, q, out, in_, r, w, **kw):
        return self.op(q, lambda e: e.dma_start(out=out, in_=in_, **kw), r, w, dma=True)

    def mm(self, out, lhsT, rhs, start, stop, r, w):
        return self.op("pe", lambda e: e.matmul(out, lhsT, rhs, start=start, stop=stop), r, w)

    def mmk(self, out, pairs, r, w):
        n = len(pairs)

        def fn(e):
            inst = None
            for i, (a, b) in enumerate(pairs):
                inst = e.matmul(out, a, b, start=(i == 0), stop=(i == n - 1))
            return inst
        return self.op("pe", fn, r, w)

    def act(self, out, in_, func, r, w, bias=None, scale=None, accum_out=None):
        kw = {}
        if bias is not None:
            kw["bias"] = bias
        if scale is not None:
            kw["scale"] = scale
        if accum_out is not None:
            kw["accum_out"] = accum_out
        return self.op("act", lambda e: e.activation(out=out, in_=in_, func=func, **kw), r, w, fs=(0 if accum_out is not None else self.fsz(out)))

    def tt(self, out, in0, in1, op, r, w, eng="dve"):
        return self.op(eng, lambda e: e.tensor_tensor(out=out, in0=in0, in1=in1, op=op), r, w, fs=self.fsz(out))

    def ts(self, out, in0, s1, s2, op0, op1, r, w, eng="dve"):
        if op1 is None:
            return self.op(eng, lambda e: e.tensor_scalar(out=out, in0=in0, scalar1=s1, scalar2=None, op0=op0), r, w, fs=self.fsz(out))
        return self.op(eng, lambda e: e.tensor_scalar(out=out, in0=in0, scalar1=s1, scalar2=s2, op0=op0, op1=op1), r, w, fs=self.fsz(out))

    def stt(self, out, in0, scalar, in1, op0, op1, r, w):
        return self.op("dve", lambda e: e.scalar_tensor_tensor(out=out, in0=in0, scalar=scalar, in1=in1, op0=op0, op1=op1), r, w, fs=self.fsz(out))

    def cp(self, eng, out, in_, r, w):
        if eng == "act":
            return self.op("act", lambda e: e.copy(out=out, in_=in_), r, w, fs=self.fsz(out))
        return self.op(eng, lambda e: e.tensor_copy(out=out, in_=in_), r, w, fs=self.fsz(out))

    def wconv(self, name, src3, kc, n):
        if name in self.dram:
            return self.dram[name]
        t = self.dint(name, [P, kc, n], BF16)
        self.dma("pool", t[:, :, :], src3, r=(), w=[("dram", name)])
        return t

    def slab_load(self, name, kc, n):
        i = self.slabs.get()
        dst = self.slab_t[i][:, 0:kc, 0:n]
        self.dma("sp", dst, self.dram[name][:, :, :], r=[("dram", name)], w=[("slab", i)])
        return i

    def li(self, l):
        return l if self.mode == "fused" else 0

    def win_src(self, l, c0, n):
        return self.dram["w_in"][self.li(l), :, c0:c0 + n].rearrange("(kc p) n -> p kc n", p=P)

    def build(self):
        nc = self.nc
        ph = self.phases
        fused = self.mode == "fused"
        if fused:
            self.din("xlay", [NTF * TT, D])
            self.din("keep", [P, NTF])
        elif "L0P1" in ph or "L0P2" in ph:
            self.din("x", [SEG, D])
            self.din("halo0", [4, D])
        if not fused:
            self.din("masks", [P, 8])
            self.din("hmask", [P, 8])
        self.din("consts", [P, 1024])
        self.din("pvec", [P, 2 * NPV])
        self.din("wbd", [2, 2, P, 4, P])
        nl = 2 if fused else 1
        need = {"w_in": [nl, D, DIN], "gla_wa": [2, 16, 256]}
        if fused or "L0P2" in ph or "L1P2" in ph:
            need.update({"w_branch": [nl, 3, 512, D], "w_out": [nl, D, D], "ln1_g": [2, D], "ln1_b": [2, D], "ln2_g": [2, D], "ln2_b": [2, D]})
        if fused or "L0P2" in ph:
            need.update({"ffn_wg": [1, D, 2816], "ffn_wu": [1, D, 2816], "ffn_wd": [1, 2816, D]})
        if fused or "L1P2" in ph:
            need.update({"router_w": [1, D, 8], "moe_wg": [1, 8, D, 3584], "moe_wu": [1, 8, D, 3584], "moe_wd": [1, 8, 3584, D]})
        else:
            need.update({"router_w": [1, D, 8]})
        for nm, shp in need.items():
            self.din(nm, shp)
        if fused:
            self.dint("r1", [NTF * TT, D])
            self.dout("y", [SEG, D])
        else:
            if "L0P1" in ph:
                self.dout("st0", [ST_ROWS, P])
            if "L0P2" in ph:
                self.din("st0_all", [8 * ST_ROWS, P])
                self.dout("r1", [SEG, D])
            if "L1P1" in ph:
                self.din("r1", [SEG, D])
                self.din("hl_all", [32, D])
                self.dout("st1", [ST_ROWS, P])
            if "L1P2" in ph:
                self.din("r1", [SEG, D])
                self.din("hl_all", [32, D])
                self.din("st1_all", [8 * ST_ROWS, P])
                self.dout("y", [SEG, D])

        sb = self.sb
        self.consts = sb("consts_sb", [P, 1024])
        self.ident_bf = sb("ident_bf", [P, P], BF16)
        self.causal = sb("causal", [P, 2, 256], BF16)
        self.ones_g = sb("ones_g", [P, P], BF16)
        self.ones_h = sb("ones_h", [P, P], BF16)
        self.pv = sb("pv", [P, 2 * NPV])
        self.dv = sb("dv", [P, 2, 32])
        self.keep = sb("keep_sb", [P, NTF])
        self.mk = sb("mk", [P, 8])
        self.hm = sb("hm", [P, 8])
        self.lnp = sb("lnp", [P, 4, D])
        self.wbd = sb("wbd_bf", [P, 2, 4, P], BF16)
        self.wa = sb("wa_bf", [16, 256], BF16)
        self.rw = sb("rw", [P, 8, 8])
        self.St = sb("S_state", [P, 6, P])
        self.Sbf = sb("S_bf", [P, 2, 6, P], BF16)
        self.dch = sb("dch", [P, 6, 8])
        self.gsum = sb("gsum", [P, 6])
        self.gtmp = sb("gtmp", [P, 6])
        self.rsum = sb("rsum", [P, 4])
        self.rtmp = sb("rtmp", [P, 4])
        self.hc = sb("hcarry", [P, 4])
        if not fused:
            self.misc = sb("misc", [P, P])
            self.stl = sb("stl", [P, 7, P])
            self.deff = sb("deff", [P, 16])
            self.hal = sb("hal", [4, D])
            self.halj = sb("halj", [4, D])
        self.xTh = sb("xTh", [P, 8, 4], BF16)
        self.xtok_b = [sb("xtokA", [P, 4, D])] + ([sb("xtokB", [P, 4, D])] if fused else [])
        self.xi = 0
        self.xtok = self.xtok_b[0]
        self.xT = sb("xT", [P, 8, TT], BF16)
        self.x1T = sb("x1T", [P, 8, TT], BF16)
        self.slab_t = [sb(f"slab{i}", [P, 8, 512], BF16) for i in range(4 if fused else 3)]
        self.slabs = Ring(self.slab_t)
        self.uext = sb("uext", [P, 4, 4 + TT])
        self.yT = sb("yT", [P, 12, TT], BF16)
        self.qt = sb("qt", [P, 4, TT], BF16)
        self.kt = sb("kt", [P, 4, TT], BF16)
        self.kd = sb("kdT", [P, 4, TT], BF16)
        self.kdtok = sb("kdtok", [P, 4, 4, P], BF16)
        self.vtok = sb("vtok", [P, 4, 512], BF16)
        self.Asb = sb("Asb", [P, 2, 4, 256], BF16)
        self.mixT = sb("mixT", [P, 8, TT], BF16)
        self.hT = sb("hT", [P, 28, TT], BF16)
        self.alow = sb("alow", [16, TT], BF16)
        self.lnst = sb("lnst", [P, 4, 2, 6])
        self.lnmv = sb("lnmv", [P, 4, 2])
        self.lnr = sb("lnr", [P, 4, 2])
        self.rl = sb("rl", [P, 4, 8])
        self.rm8 = sb("rm8", [P, 4, 8])
        self.rwt = sb("rwt", [P, 4, 8])
        self.rsm = sb("rsm", [P, 4, 4])
        self.ring_t = [sb(f"ring{i}", [P, TT]) for i in range(7 if fused else 8)]
        self.ring = Ring(self.ring_t)
        self.ps_t = [self.es.enter_context(nc.psum_tensor(f"ps{i}", [P, 512], F32)) for i in range(8)]
        self.ps = Ring(self.ps_t)

        sem = lambda n: self.es.enter_context(nc.semaphore(n))
        self.sems = {e: sem("s_" + e) for e in ("pe", "act", "dve", "pool")}
        self.sems["dma_sp"] = [sem(f"d_sp{i}") for i in range(N_SP_SEMS)]
        self.sems["dma_pool"] = [sem(f"d_pl{i}") for i in range(N_POOL_SEMS)]

        self.prologue()
        if fused:
            for l in (0, 1):
                self.conv_win(l, ["rg", "ga", "qk", "gv", "hf", "hi"])
                self.conv_win(l, ["gg", "hq", "hg"] + [f"gt{i}" for i in range(6)])
                self.conv_layer_rest(l)
            self.fused_program()
        else:
            for p in ph:
                if p == "L0P1":
                    self.layer_pass(0, 1)
                elif p == "L0P2":
                    self.layer_pass(0, 2)
                elif p == "L1P1":
                    self.layer_pass(1, 1)
                elif p == "L1P2":
                    self.layer_pass(1, 2)
        self.op("sp", None, r=[("out", n) for n in self.out_keys()], w=())

        self.S.finalize(self.sems)
        S = self.S
        with nc.Block() as block:
            @block.sync
            def _(e):
                S.emit("sp", e)

            @block.tensor
            def _(e):
                S.emit("pe", e)

            @block.scalar
            def _(e):
                S.emit("act", e)

            @block.vector
            def _(e):
                S.emit("dve", e)

            @block.gpsimd
            def _(e):
                S.emit("pool", e)
        self.es.close()
        return nc

    def out_keys(self):
        return list(self._outk)

    def prologue(self):
        self._outk = []
        d = self.dram
        c = self.consts
        self.dma("sp", c[:], d["consts"][:, :], (), ["consts"])
        self.dma("sp", self.pv[:], d["pvec"][:, :], (), ["pv"])
        if self.mode == "fused":
            self.dma("sp", self.keep[:], d["keep"][:, :], (), ["keep"])
        else:
            self.dma("sp", self.mk[:], d["masks"][:, :], (), ["mk"])
            self.dma("sp", self.hm[:], d["hmask"][:, :], (), ["hm"])
        self.dma("sp", self.rw[:], d["router_w"][0].rearrange("(kc p) e -> p kc e", p=P), (), ["rw"])
        self.cp("dve", self.ident_bf[:], c[:, 0:128], ["consts"], ["ident"])
        self.op("dve", lambda e: e.memset(self.causal[:], 0.0), (), ["causal"])
        self.cp("dve", self.causal[0:64, 0, :], c[0:64, 640:896], ["consts", "causal"], ["causal"])
        self.cp("dve", self.causal[64:128, 1, :], c[64:128, 640:896], ["consts", "causal"], ["causal"])
        self.ts(self.ones_g[:], c[:, 896:1024], 1.0 / 128, None, ALU.mult, None, ["consts"], ["ones_g"])
        self.ts(self.ones_h[:], c[:, 896:1024], 1.0 / 512, None, ALU.mult, None, ["consts"], ["ones_h"])
        for l in (0, 1):
            pv = self.pv[:, l * NPV:(l + 1) * NPV]
            dv = self.dv[:, l, :]
            self.act(dv[:, 0:4], pv[:, 28:32], AF.Exp, ["pv"], [("dv", l, 0)], scale=-1.0)
            self.act(dv[:, 0:4], dv[:, 0:4], AF.Ln, [("dv", l, 0)], [("dv", l, 0)], bias=1.0)
            self.ts(dv[:, 4:8], dv[:, 0:4], -16.0, None, ALU.mult, None, [("dv", l, 0)], [("dv", l, 1)])
            self.ts(dv[:, 0:4], dv[:, 0:4], -8.0, None, ALU.mult, None, [("dv", l, 0), ("dv", l, 1)], [("dv", l, 0)])
            self.ts(dv[:, 8:10], pv[:, 32:34], -1.0, None, ALU.mult, None, ["pv"], [("dv", l, 2)])
            if l == 0:
                self.op("dve", lambda e, o=dv[:, 10:14]: e.memset(o, 0.0), (), [("dv", l, 3)])
            else:
                self.tt(dv[:, 10:14], pv[:, 39:43], pv[:, 35:39], ALU.subtract, ["pv"], [("dv", l, 3)])
                self.act(dv[:, 10:14], dv[:, 10:14], AF.Sigmoid, [("dv", l, 3)], [("dv", l, 3)])
            self.ts(dv[:, 14:18], dv[:, 10:14], -1.0, 1.0, ALU.mult, ALU.add, [("dv", l, 3)], [("dv", l, 4)])
            self.ts(dv[:, 18:22], dv[:, 10:14], -1.0, None, ALU.add, None, [("dv", l, 3)], [("dv", l, 5)])

    def conv_win(self, l, names):
        tab = {"rg": (C_RG, 512), "qk": (C_GQ, 512), "gv": (C_GV, 512), "ga": (C_GA, 16), "gg": (C_GG, 512),
               "hq": (C_HQ, 512), "hf": (C_HF, 512), "hi": (C_HI, 512), "hg": (C_HG, 512)}
        for i in range(6):
            tab[f"gt{i}"] = (C_GATES + 512 * i, 512)
        for nm in names:
            c0, n = tab[nm]
            self.wconv(f"win{l}_{nm}", self.win_src(l, c0, n), 8, n)

    def conv_layer_rest(self, l):
        d = self.dram
        for n in range(3):
            for dh in range(2):
                self.wconv(f"wbr{l}_{n}_{dh}", d["w_branch"][self.li(l), n, :, dh * 512:(dh + 1) * 512].rearrange("(kc p) n -> p kc n", p=P), 4, 512)
        for dh in range(2):
            self.wconv(f"wout{l}_{dh}", d["w_out"][self.li(l), :, dh * 512:(dh + 1) * 512].rearrange("(kc p) n -> p kc n", p=P), 8, 512)
        if l == 0:
            for g in range(6):
                n = 512 if g < 5 else 256
                self.wconv(f"fg_{g}", d["ffn_wg"][0, :, g * 512:g * 512 + n].rearrange("(kc p) n -> p kc n", p=P), 8, n)
                self.wconv(f"fu_{g}", d["ffn_wu"][0, :, g * 512:g * 512 + n].rearrange("(kc p) n -> p kc n", p=P), 8, n)
            for dh in range(2):
                for g in range(6):
                    nf = 4 if g < 5 else 2
                    self.wconv(f"fd_{g}_{dh}", d["ffn_wd"][0, g * 512:g * 512 + nf * P, dh * 512:(dh + 1) * 512].rearrange("(fc p) n -> p fc n", p=P), nf, 512)
        else:
            for e in range(8):
                for g in range(7):
                    self.wconv(f"mg_{e}_{g}", d["moe_wg"][0, e, :, g * 512:(g + 1) * 512].rearrange("(kc p) n -> p kc n", p=P), 8, 512)
                    self.wconv(f"mu_{e}_{g}", d["moe_wu"][0, e, :, g * 512:(g + 1) * 512].rearrange("(kc p) n -> p kc n", p=P), 8, 512)
                for dh in range(2):
                    for g in range(7):
                        self.wconv(f"md_{e}_{g}_{dh}", d["moe_wd"][0, e, g * 512:(g + 1) * 512, dh * 512:(dh + 1) * 512].rearrange("(fc p) n -> p fc n", p=P), 4, 512)

    def layer_consts(self, l, full):
        d = self.dram
        R = self.ring
        i0 = R.get()
        wa_f = self.ring_t[i0][0:16, 0:256]
        self.dma("sp", wa_f, d["gla_wa"][l], (), [("ring", i0)])
        self.cp("dve", self.wa[:], wa_f, [("ring", i0)], ["wa"])
        R.put(i0)
        for j in range(2):
            i1 = R.get()
            wbd_f = self.ring_t[i1][:].rearrange("p (c m) -> p c m", m=P)
            self.dma("sp", wbd_f, d["wbd"][l, j], (), [("ring", i1)])
            self.cp("dve", self.wbd[:, j], wbd_f, [("ring", i1)], [("wbd", j)])
            R.put(i1)
        if full:
            for j, nm in enumerate(("ln1_g", "ln1_b", "ln2_g", "ln2_b")):
                self.dma("sp", self.lnp[:, j, :], d[nm][l:l + 1, :].broadcast_to([P, D]), (), [("lnp", j)])

    def transpose_tok(self, src, src_keys, dstT, dst_key):
        for kc in range(8):
            b = self.ps.get()
            pf = self.ps_t[b]

            def fn(e, kc=kc, pf=pf):
                inst = None
                for blk in range(4):
                    inst = e.transpose(pf[:, blk * P:(blk + 1) * P], src[:, blk, kc * P:(kc + 1) * P], self.consts[:, 0:128])
                return inst
            self.op("pe", fn, list(src_keys) + ["consts"], [("ps", b)])
            eng = "act" if kc % 2 == 0 else "dve"
            self.cp(eng, dstT[:, kc, :], pf[:, 0:TT], [("ps", b)], [(dst_key, kc)])
            self.ps.put(b)

    def proj_fm(self, slab, xT, xkey, col0, M=P, N=TT, ncols=None):
        b = self.ps.get()
        st = self.slab_t[slab]
        out = self.ps_t[b][0:M, 0:N]
        pairs = [(st[:, kc, col0:col0 + M], xT[:, kc, 0:N]) for kc in range(8)]
        self.mmk(out, pairs, [("slab", slab)] + [(xkey, kc) for kc in range(8)], [("ps", b)])
        return b

    def proj_tm(self, slab, xT, xkey, blk, n=512):
        b = self.ps.get()
        st = self.slab_t[slab]
        out = self.ps_t[b][:, 0:n]
        pairs = [(xT[:, kc, blk * P:(blk + 1) * P], st[:, kc, 0:n]) for kc in range(8)]
        self.mmk(out, pairs, [("slab", slab)] + [(xkey, kc) for kc in range(8)], [("ps", b)])
        return b

    def layer_pass(self, l, pas):
        d = self.dram
        full = pas == 2
        fused = self.mode == "fused"
        res_in = d["x"] if l == 0 else d["r1"]
        self.resname = "x" if l == 0 else "r1"
        res_out = None
        if full:
            res_out = d["r1"] if l == 0 else d["y"]
        if pas == 1 or not fused:
            self.conv_win(l, ["rg", "ga", "qk", "gv", "hf", "hi"])
        if full:
            self.conv_win(l, ["gg", "hq", "hg"] + [f"gt{i}" for i in range(6)])
            self.conv_layer_rest(l)
        self.layer_consts(l, full)
        pv = self.pv[:, l * NPV:(l + 1) * NPV]
        dv = self.dv[:, l, :]
        DVK = [("dv", l, i) for i in range(6)] + ["pv"]

        z = lambda ap, keys: self.op("dve", lambda e: e.memset(ap, 0.0), (), keys)
        z(self.St[:], [("S", b) for b in range(6)])
        z(self.hc[:], ["hc"])
        if not full:
            z(self.gsum[:], ["gsum"])
            z(self.rsum[:], ["rsum"])
        else:
            self.combine_states(l)
        self.halo(l)

        if self.dbg.get("stop") == "pre":
            self.dump("dbg_S", self.St[:], [P, 6, P], [("S", b) for b in range(6)])
            return
        for t in range(self.dbg.get("ntiles", NT)):
            if self.tile(l, t, full, res_in, res_out, pv, dv, DVK) == "stop":
                return

        if not full:
            self.write_states(l, dv, DVK)
        elif l == 0 and (fused or True):
            pass

    def zero_states(self):
        z = lambda ap, keys: self.op("dve", lambda e: e.memset(ap, 0.0), (), keys)
        z(self.St[:], [("S", b) for b in range(6)])
        z(self.hc[:], ["hc"])
        z(self.gsum[:], ["gsum"])
        z(self.rsum[:], ["rsum"])
        z(self.uext[:, :, 0:4], [("uext", c) for c in range(4)])
        self.cp("act", self.Sbf[:, 0, :, :], self.St[:], [("S", b) for b in range(6)], [("Sbf", 0, b) for b in range(6)])
        self.halo_pending = False

    def mask_states(self, t, full):
        k = self.keep[:, t:t + 1]
        SK = [("S", b) for b in range(6)]
        self.ts(self.St[:], self.St[:], k, None, ALU.mult, None, SK + ["keep"], SK)
        self.ts(self.hc[:], self.hc[:], k, None, ALU.mult, None, ["hc", "keep"], ["hc"])
        UK = [("uext", c) for c in range(4)]
        self.ts(self.uext[:, :, 0:4], self.uext[:, :, 0:4], k, None, ALU.mult, None, UK + ["keep"], UK)
        if full:
            self.cp("act", self.Sbf[:, 0, :, :], self.St[:], SK, [("Sbf", 0, b) for b in range(6)])

    def fused_program(self):
        d = self.dram
        tiles = []
        for t in range(NTF):
            tiles.append(dict(l=0, t=t, full=True, rin="xlay", rout="r1", out_t=t, final=False, mask=t < NPRE, first=(t == 0), own0=False))
        for t in range(NPRE):
            tiles.append(dict(l=1, t=t, full=False, rin="r1", rout=None, out_t=None, final=False, mask=True, first=(t == 0), own0=False))
        for t in range(NPRE, NTF):
            tiles.append(dict(l=1, t=t, full=True, rin="r1", rout="y", out_t=t - NPRE, final=True, mask=False, first=False, own0=(t == NPRE)))

        def xload(i):
            td = tiles[i]
            k = i % 2
            self.dma("sp", self.xtok_b[k][:], d[td["rin"]][td["t"] * TT:(td["t"] + 1) * TT, :].rearrange("(b p) d -> p b d", p=P),
                     [("dram", "res%d" % td["l"], td["t"])], [("xtok", k, b) for b in range(4)])
        xload(0)
        for i, td in enumerate(tiles):
            l = td["l"]
            pv = self.pv[:, l * NPV:(l + 1) * NPV]
            dv = self.dv[:, l, :]
            DVK = [("dv", l, j) for j in range(6)] + ["pv"]
            if td["first"]:
                self.layer_consts(l, True)
                self.zero_states()
            if td["own0"]:
                SK = [("S", b) for b in range(6)]
                self.cp("act", self.Sbf[:, 0, :, :], self.St[:], SK, [("Sbf", 0, b) for b in range(6)])
            if i + 1 < len(tiles):
                xload(i + 1)
            self.xi = i % 2
            self.xtok = self.xtok_b[self.xi]
            self.tile(l, td["t"], td["full"], d[td["rin"]], d[td["rout"]] if td["rout"] else None, pv, dv, DVK,
                      out_t=td["out_t"], final=td["final"])
            if td["mask"]:
                self.mask_states(td["t"], td["full"])

    def halo(self, l):
        d = self.dram
        if l == 0:
            self.dma("sp", self.hal[:], d["halo0"][:, :], (), ["hal"])
        else:
            for j in range(8):
                self.dma("sp", self.halj[:], d["hl_all"][4 * j:4 * j + 4, :], [("dram", "hl_all")], ["halj"])
                if j == 0:
                    self.ts(self.hal[:], self.halj[:], self.hm[0:4, 0:1], None, ALU.mult, None, ["halj", "hm"], ["hal"])
                else:
                    self.stt(self.hal[:], self.halj[:], self.hm[0:4, j:j + 1], self.hal[:], ALU.mult, ALU.add, ["halj", "hm", "hal"], ["hal"])
        b = self.ps.get()
        pf = self.ps_t[b]

        def fn(e):
            inst = None
            for kc in range(8):
                inst = e.transpose(pf[:, kc * 4:(kc + 1) * 4], self.hal[0:4, kc * P:(kc + 1) * P], self.consts[0:4, 0:4])
            return inst
        self.op("pe", fn, ["hal", "consts"], [("ps", b)])
        self.cp("dve", self.xTh[:].rearrange("p k t -> p (k t)"), pf[:, 0:32], [("ps", b)], [("xTh", kc) for kc in range(8)])
        self.ps.put(b)
        self.halo_pending = True

    def write_states(self, l, dv, DVK):
        d = self.dram
        name = f"st{l}"
        st = d[name]
        z = lambda ap, keys: self.op("dve", lambda e: e.memset(ap, 0.0), (), keys)
        z(self.misc[:], ["misc"])
        self.act(self.misc[:, 0:2], self.gsum[:, 0:2], AF.Exp, ["gsum"], ["misc"], scale=-1.0 / 16)
        self.act(self.misc[:, 2:6], self.gsum[:, 2:6], AF.Exp, ["gsum"], ["misc"], scale=1.0)
        self.tt(self.rtmp[:], self.rsum[:], dv[:, 0:4], ALU.mult, ["rsum"] + DVK, ["rtmp"])
        self.act(self.misc[:, 6:10], self.rtmp[:], AF.Exp, ["rtmp"], ["misc"])
        self.cp("dve", self.misc[:, 10:14], self.hc[:], ["hc", "misc"], ["misc"])
        k1 = self.dma("sp", st[0:6 * P, :].rearrange("(b p) c -> p b c", p=P), self.St[:], [("S", b) for b in range(6)], [("dram", name), ("out", name + "a")])
        k2 = self.dma("sp", st[6 * P:7 * P, :], self.misc[:], ["misc"], [("dram", name + "m"), ("out", name + "b")])
        self._outk += [name + "a", name + "b"]

    def combine_states(self, l):
        d = self.dram
        alln = d[f"st{l}_all"]
        for j in range(8):
            self.dma("sp", self.stl[:], alln[j * ST_ROWS:(j + 1) * ST_ROWS, :].rearrange("(b p) c -> p b c", p=P),
                     [("dram", f"st{l}_all")], ["stl"])
            mj = self.mk[:, j:j + 1]
            self.ts(self.deff[:, 0:10], self.stl[:, 6, 0:10], -1.0, mj, ALU.add, ALU.mult, ["stl", "mk"], ["deff"])
            self.ts(self.deff[:, 0:10], self.deff[:, 0:10], 1.0, None, ALU.add, None, ["deff"], ["deff"])
            self.ts(self.stl[:, 0:6, :], self.stl[:, 0:6, :], mj, None, ALU.mult, None, ["stl", "mk"], ["stl"])
            self.ts(self.stl[:, 6, 10:14], self.stl[:, 6, 10:14], mj, None, ALU.mult, None, ["stl", "mk"], ["stl"])
            for b in range(6):
                self.stt(self.St[:, b, :], self.St[:, b, :], self.deff[:, b:b + 1], self.stl[:, b, :], ALU.mult, ALU.add,
                         ["stl", "deff", ("S", b)], [("S", b)])
            self.tt(self.hc[:], self.hc[:], self.deff[:, 6:10], ALU.mult, ["hc", "deff"], ["hc"])
            self.tt(self.hc[:], self.hc[:], self.stl[:, 6, 10:14], ALU.add, ["hc", "stl"], ["hc"])
        self.cp("act", self.Sbf[:, 0, :, :], self.St[:], [("S", b) for b in range(6)], [("Sbf", 0, b) for b in range(6)])

    def allgather(self, p):
        d = self.dram
        nm = {"AG0": ("st0", "st0_all"), "AG1": ("st1", "st1_all"), "AGH": ("hl", "hl_all")}[p]
        src, dst = d[nm[0]], d[nm[1]]
        rk = [("dram", nm[0])] + ([("dram", nm[0] + "m")] if p != "AGH" else [])
        self.op("pool", lambda e: e.collective_compute("AllGather", ALU.bypass, replica_groups=[list(range(8))],
                                                       ins=[src[:, :]], outs=[dst[:, :]]), rk, [("dram", nm[1])], dma=True)

    def tile(self, l, t, full, res_in, res_out, pv, dv, DVK, out_t=None, final=True):
        fused = self.mode == "fused"
        if out_t is None:
            out_t = t
        XK = [("xtok", self.xi, b) for b in range(4)]
        if not fused:
            self.dma("sp", self.xtok[:], res_in[t * TT:(t + 1) * TT, :].rearrange("(b p) d -> p b d", p=P),
                     [("dram", "res%d" % l, t)], XK)
        self.transpose_tok(self.xtok, XK, self.xT, "xT")
        stop = self.dbg.get("stop")
        YK = [("yT", i) for i in range(12)]
        self.rg_branch(l, t, full, pv, dv, DVK)
        if stop == "rg":
            self.dump("dbg_y", self.yT[:, 0:4, :], [P, 4, TT], YK[0:4], BF16)
            return "stop"
        self.attn_branch(l, t, full, "gla", pv, dv, DVK)
        if stop == "gla":
            self.dump("dbg_y", self.yT[:, 0:8, :], [P, 8, TT], YK[0:8], BF16)
            return "stop"
        self.attn_branch(l, t, full, "hg", pv, dv, DVK)
        if not full:
            return
        if stop == "hg":
            self.dump("dbg_y", self.yT[:], [P, 12, TT], YK, BF16)
            return "stop"
        self.merge_out(l, t)
        if stop == "merge":
            self.dump("dbg_x", self.xtok[:], [P, 4, D], XK)
            return "stop"
        self.layernorm(0)
        if stop == "ln1":
            self.dump("dbg_x", self.xtok[:], [P, 4, D], XK)
            return "stop"
        self.transpose_tok(self.xtok, XK, self.x1T, "x1T")
        if l == 0:
            self.ffn_dense()
        else:
            self.moe()
        self.layernorm(2)
        wk = [("dram", "res%d" % (l + 1), t)]
        if final:
            wk.append(("out", "res%d_%d" % (l, t)))
            self._outk.append("res%d_%d" % (l, t))
        self.dma("sp", res_out[out_t * TT:(out_t + 1) * TT, :].rearrange("(b p) d -> p b d", p=P), self.xtok[:], XK, wk)

    def rg_branch(self, l, t, full, pv, dv, DVK):
        sl = self.slab_load(f"win{l}_rg", 8, 512)
        if self.halo_pending:
            for c in range(4):
                b = self.proj_fm(sl, self.xTh, "xTh", c * P, N=4)
                self.cp("dve", self.uext[:, c, 0:4], self.ps_t[b][:, 0:4], [("ps", b)], [("uext", c)])
                self.ps.put(b)
            self.halo_pending = False
        for c in range(4):
            b = self.proj_fm(sl, self.xT, "xT", c * P)
            self.cp("act", self.uext[:, c, 4:4 + TT], self.ps_t[b][:, :], [("ps", b)], [("uext", c)])
            self.ps.put(b)
        self.slabs.put(sl)
        for c in range(4):
            R = self.ring
            ic = R.get()
            cc = self.ring_t[ic]
            ck = ("ring", ic)
            self.ts(cc[:], self.uext[:, c, 4:4 + TT], pv[:, c * 4 + 3:c * 4 + 4], pv[:, 16 + c:17 + c], ALU.mult, ALU.add,
                    [("uext", c), "pv"], [ck])
            for j in range(3):
                self.stt(cc[:], self.uext[:, c, 1 + j:1 + j + TT], pv[:, c * 4 + j:c * 4 + j + 1], cc[:], ALU.mult, ALU.add,
                         [("uext", c), "pv", ck], [ck])
            self.cp("dve", self.uext[:, c, 0:4], self.uext[:, c, TT:TT + 4], [("uext", c)], [("uext", c)])
            ib = R.get()
            ccb = self.ring_t[ib][:].bitcast(BF16)[:, 0:TT]
            self.cp("act", ccb, cc[:], [ck], [("ring", ib)])
            br = self.ps.get()
            self.mm(self.ps_t[br][:, :], self.wbd[:, 0, c, :], ccb, True, True, [("wbd", 0), ("ring", ib)], [("ps", br)])
            bi = self.ps.get()
            self.mm(self.ps_t[bi][:, :], self.wbd[:, 1, c, :], ccb, True, True, [("wbd", 1), ("ring", ib)], [("ps", bi)])
            R.put(ib)
            ir = R.get()
            r = self.ring_t[ir]
            if full:
                self.act(r[:], self.ps_t[br][:, :], AF.Sigmoid, [("ps", br), "pv"], [("ring", ir)], bias=pv[:, 20 + c:21 + c])
            else:
                self.act(r[:], self.ps_t[br][:, :], AF.Sigmoid, [("ps", br), "pv"], [("ring", ir), "rtmp"], bias=pv[:, 20 + c:21 + c],
                         accum_out=self.rtmp[:, c:c + 1])
                self.tt(self.rsum[:, c:c + 1], self.rsum[:, c:c + 1], self.rtmp[:, c:c + 1], ALU.add, ["rtmp", "rsum"], ["rsum"])
            self.ps.put(br)
            ii = R.get()
            gi = self.ring_t[ii]
            self.act(gi[:], self.ps_t[bi][:, :], AF.Sigmoid, [("ps", bi), "pv"], [("ring", ii)], bias=pv[:, 24 + c:25 + c])
            self.ps.put(bi)
            self.tt(gi[:], gi[:], cc[:], ALU.mult, [("ring", ii), ck], [("ring", ii)])
            R.put(ic)
            ia = R.get()
            a = self.ring_t[ia]
            self.act(a[:], r[:], AF.Exp, [("ring", ir)] + DVK, [("ring", ia)], scale=dv[:, c:c + 1])
            self.act(r[:], r[:], AF.Exp, [("ring", ir)] + DVK, [("ring", ir)], scale=dv[:, 4 + c:5 + c])
            self.act(r[:], r[:], AF.Sqrt, [("ring", ir)], [("ring", ir)], scale=-1.0, bias=1.0)
            self.tt(gi[:], gi[:], r[:], ALU.mult, [("ring", ii), ("ring", ir)], [("ring", ii)])
            self.op("dve", lambda e, o=r[:], a_=a[:], b_=gi[:], h0=self.hc[:, c:c + 1]: e.tensor_tensor_scan(
                out=o, data0=a_, data1=b_, initial=h0, op0=ALU.mult, op1=ALU.add),
                [("ring", ia), ("ring", ii), "hc"], [("ring", ir)])
            self.cp("dve", self.hc[:, c:c + 1], r[:, TT - 1:TT], [("ring", ir), "hc"], ["hc"])
            R.put(ia)
            R.put(ii)
            if full:
                self.cp("pool", self.yT[:, c, :], r[:], [("ring", ir)], [("yT", c)])
            R.put(ir)

    def _dep_of(self, key):
        w = self.S.lastw.get(key)
        return {w} if w is not None else set()

    def attn_branch(self, l, t, full, kind, pv, dv, DVK):
        R = self.ring
        gla = kind == "gla"
        nb = 2 if gla else 4
        sb0 = 0 if gla else 2
        sgn = (-1.0 / 16) if gla else 1.0
        qscale = (64 ** -0.5) if gla else (128 ** -0.5)
        Kh = 64 if gla else 128
        heads = [(h // 2, (h % 2) * 64) for h in range(4)] if gla else [(h, 0) for h in range(4)]
        sl = self.slab_load(f"win{l}_" + ("gv" if gla else "hi"), 8, 512)
        for blk in range(4):
            b = self.proj_tm(sl, self.xT, "xT", blk)
            self.cp("act" if blk % 2 else "dve", self.vtok[:, blk, :], self.ps_t[b][:, :], [("ps", b)], [("vtok", blk)])
            self.ps.put(b)
        self.slabs.put(sl)
        if gla:
            sla = self.slab_load(f"win{l}_ga", 8, 16)
            b = self.proj_fm(sla, self.xT, "xT", 0, M=16)
            self.cp("act", self.alow[:], self.ps_t[b][0:16, :], [("ps", b)], ["alow"])
            self.ps.put(b)
            self.slabs.put(sla)
            slq = self.slab_load(f"win{l}_qk", 8, 512)
        else:
            slf = self.slab_load(f"win{l}_hf", 8, 512)
            slq = self.slab_load(f"win{l}_hq", 8, 512) if full else None
        for fb in range(nb):
            ig = R.get()
            G = self.ring_t[ig]
            gk = ("ring", ig)
            ikk = None
            if gla:
                b = self.ps.get()
                self.mm(self.ps_t[b][:, :], self.wa[0:16, fb * P:(fb + 1) * P], self.alow[0:16, :], True, True, ["wa", "alow"], [("ps", b)])
                self.act(G[:], self.ps_t[b][:, :], AF.Exp, [("ps", b)] + DVK, [gk], scale=-1.0, bias=dv[:, 8 + fb:9 + fb])
                self.ps.put(b)
                self.act(G[:], G[:], AF.Ln, [gk], [gk], bias=1.0)
            else:
                b = self.proj_fm(slf, self.xT, "xT", fb * P)
                ikk = R.get()
                kk = self.ring_t[ikk]
                self.act(kk[:], self.ps_t[b][:, :], AF.Sigmoid, [("ps", b)], [("ring", ikk)])
                self.ps.put(b)
                self.ts(G[:], kk[:], dv[:, 14 + fb:15 + fb], dv[:, 10 + fb:11 + fb], ALU.mult, ALU.add, [("ring", ikk)] + DVK, [gk])
                self.act(G[:], G[:], AF.Ln, [gk], [gk])
                self.ts(kk[:], kk[:], dv[:, 18 + fb:19 + fb], dv[:, 14 + fb:15 + fb], ALU.mult, ALU.add, [("ring", ikk)] + DVK, [("ring", ikk)])
            ie = R.get()
            Gc = self.ring_t[ie]
            self.op("dve", lambda e, o=Gc[:], i_=G[:], m=self.consts[:, 128:640]: e.tensor_tensor_scan(
                out=o, data0=m, data1=i_, initial=0.0, op0=ALU.mult, op1=ALU.add), [gk, "consts"], [("ring", ie)])
            ig, ie = ie, ig
            G, gk = Gc, ("ring", ig)
            G3 = G[:].rearrange("p (c s) -> p c s", s=64)
            sb = sb0 + fb
            self.act(self.dch[:, sb, :], G3[:, :, 63], AF.Exp, [gk], [("dch", sb)], scale=sgn)
            if not full:
                self.op("dve", lambda e, o=self.gtmp[:, sb:sb + 1], i_=G3[:, :, 63]: e.tensor_reduce(out=o, in_=i_, axis=AX.X, op=ALU.add),
                        [gk], ["gtmp"])
                self.tt(self.gsum[:, sb:sb + 1], self.gsum[:, sb:sb + 1], self.gtmp[:, sb:sb + 1], ALU.add, ["gtmp", "gsum"], ["gsum"])
            E = self.ring_t[ie]
            ek = ("ring", ie)
            E3 = E[:].rearrange("p (c s) -> p c s", s=64)
            self.tt(E3, G3[:, :, 63:64].broadcast_to([P, 8, 64]), G3, ALU.subtract, [gk], [ek])
            self.act(E[:], E[:], AF.Exp, [ek], [ek], scale=sgn)
            if gla:
                bk = self.proj_fm(slq, self.xT, "xT", 256 + fb * P)
                self.tt(self.kd[:, fb, :], self.ps_t[bk][:, :], E[:], ALU.mult, [("ps", bk), ek], [("kd", fb)])
            else:
                self.tt(self.kd[:, fb, :], kk[:], E[:], ALU.mult, [("ring", ikk), ek], [("kd", fb)])
            if full:
                self.act(E[:], G[:], AF.Exp, [gk, ek], [ek], scale=-sgn)
                if gla:
                    self.tt(self.kt[:, fb, :], self.ps_t[bk][:, :], E[:], ALU.mult, [("ps", bk), ek], [("kt", fb)])
                else:
                    self.tt(self.kt[:, fb, :], kk[:], E[:], ALU.mult, [("ring", ikk), ek], [("kt", fb)])
                self.act(E[:], G[:], AF.Exp, [gk, ek], [ek], scale=sgn)
                if gla:
                    bq = self.proj_fm(slq, self.xT, "xT", fb * P)
                    for hh in range(2):
                        qi = 2 * hh + fb
                        oth = slice((1 - hh) * 64, (2 - hh) * 64)
                        own = slice(hh * 64, (hh + 1) * 64)
                        self.op("pool", lambda e, o=self.qt[oth, qi, :]: e.memset(o, 0.0), (), [("qt", qi)])
                        self.stt(self.qt[own, qi, :], self.ps_t[bq][own, :], qscale, E[own, :], ALU.mult, ALU.mult,
                                 [("ps", bq), ek, ("qt", qi)], [("qt", qi)])
                    self.ps.put(bq)
                else:
                    bq = self.proj_fm(slq, self.xT, "xT", fb * P)
                    self.act(G[:], self.ps_t[bq][:, :], AF.Silu, [("ps", bq), gk], [gk])
                    self.ps.put(bq)
                    self.stt(self.qt[:, fb, :], G[:], qscale, E[:], ALU.mult, ALU.mult, [gk, ek], [("qt", fb)])
            if gla:
                self.ps.put(bk)
            else:
                R.put(ikk)
            R.put(ie)
            R.put(ig)
            b = self.ps.get()
            pbf = self.ps_t[b][:].bitcast(BF16)

            def fn(e, fb=fb, pbf=pbf):
                inst = None
                for blk in range(4):
                    inst = e.transpose(pbf[:, blk * P:(blk + 1) * P], self.kd[:, fb, blk * P:(blk + 1) * P], self.ident_bf[:])
                return inst
            self.op("pe", fn, [("kd", fb), "ident"], [("ps", b)])
            self.cp("act", self.kdtok[:, fb, :, :].rearrange("p b f -> p (b f)"), pbf[:, 0:512], [("ps", b)], [("kdtok", fb)])
            self.ps.put(b)
        self.slabs.put(slq) if slq is not None else None
        if not gla:
            self.slabs.put(slf)
        po = None
        if full:
            po = [self.ps.get() for _ in range(4)]
        for c in range(8):
            blk, hp = c // 2, (c % 2) * 64
            par = c % 2
            if full and c % 2 == 0:
                pa = self.ps.get()
                for cc_ in (c, c + 1):
                    hp_ = (cc_ % 2) * 64
                    for h, (fb, kp) in enumerate(heads):
                        qi = (2 * (kp // 64) + fb) if gla else fb
                        self.mm(self.ps_t[pa][hp_:hp_ + 64, h * 64:(h + 1) * 64],
                                self.kt[:, fb, cc_ * 64:(cc_ + 1) * 64], self.qt[:, qi, cc_ * 64:(cc_ + 1) * 64],
                                True, True, [("kt", fb), ("qt", qi)], [("ps", pa)])
                for pz in range(2):
                    self.tt(self.Asb[:, pz, blk, :], self.ps_t[pa][:, 0:256], self.causal[:, pz, :], ALU.mult, [("ps", pa), "causal"], [("Asb", pz, blk)])
                self.ps.put(pa)
            pu = self.ps.get()
            for h, (fb, kp) in enumerate(heads):
                if gla:
                    out = self.ps_t[pu][kp:kp + 64, fb * P:(fb + 1) * P]
                    lhs = self.kdtok[hp:hp + 64, fb, blk, kp:kp + 64]
                else:
                    out = self.ps_t[pu][:, fb * P:(fb + 1) * P]
                    lhs = self.kdtok[hp:hp + 64, fb, blk, :]
                self.mm(out, lhs, self.vtok[hp:hp + 64, blk, h * P:(h + 1) * P], True, True, [("kdtok", fb), ("vtok", blk)], [("ps", pu)])
            for fb in range(nb):
                sbk = sb0 + fb
                self.stt(self.St[:, sbk, :], self.St[:, sbk, :], self.dch[:, sbk, c:c + 1], self.ps_t[pu][:, fb * P:(fb + 1) * P], ALU.mult, ALU.add,
                         [("S", sbk), ("dch", sbk), ("ps", pu)], [("S", sbk)])
            self.ps.put(pu)
            if full:
                self.cp("dve", self.Sbf[:, 1 - par, sb0:sb0 + nb, :], self.St[:, sb0:sb0 + nb, :], [("S", sb0 + i) for i in range(nb)],
                        [("Sbf", 1 - par, sb0 + i) for i in range(nb)])
            if full:
                for h, (fb, kp) in enumerate(heads):
                    sbk = sb0 + fb
                    qi = (2 * (kp // 64) + fb) if gla else fb
                    out = self.ps_t[po[h]][:, c * 64:(c + 1) * 64]
                    self.mm(out, self.vtok[:, blk, h * P:(h + 1) * P], self.Asb[:, par, blk, h * 64:(h + 1) * 64], True, False,
                            [("vtok", blk), ("Asb", par, blk)], [("ps", po[h])])
                    self.mm(out, self.Sbf[:, par, sbk, :], self.qt[:, qi, c * 64:(c + 1) * 64], False, True,
                            [("Sbf", par, sbk), ("qt", qi)], [("ps", po[h])])
        if not full:
            return
        slg = self.slab_load(f"win{l}_" + ("gg" if gla else "hg"), 8, 512)
        nw0 = 34 if gla else 43
        sq = []
        if not gla:
            pn = self.ps.get()
        for h in range(4):
            isq = R.get()
            sqb = self.ring_t[isq][:].bitcast(BF16)[:, 0:TT]
            self.act(sqb, self.ps_t[po[h]][:, :], AF.Square, [("ps", po[h])], [("ring", isq)])
            if gla:
                pn = self.ps.get()
                self.mm(self.ps_t[pn][:, :], self.ones_g[:], sqb, True, True, ["ones_g", ("ring", isq)], [("ps", pn)])
                R.put(isq)
                irs = R.get()
                rs = self.ring_t[irs]
                self.act(rs[:], self.ps_t[pn][:, :], AF.Ln, [("ps", pn)], [("ring", irs)], bias=RMS_EPS)
                self.ps.put(pn)
                self.act(rs[:], rs[:], AF.Exp, [("ring", irs)], [("ring", irs)], scale=-0.5)
                self.gate_out(l, h, slg, po[h], rs, irs, pv[:, nw0:nw0 + 1], AF.Silu, 4 + h)
                R.put(irs)
            else:
                self.mm(self.ps_t[pn][:, :], self.ones_h[:], sqb, h == 0, h == 3, ["ones_h", ("ring", isq)], [("ps", pn)])
                sq.append(isq)
        if not gla:
            for isq in sq:
                R.put(isq)
            irs = R.get()
            rs = self.ring_t[irs]
            self.act(rs[:], self.ps_t[pn][:, :], AF.Ln, [("ps", pn)], [("ring", irs)], bias=RMS_EPS)
            self.ps.put(pn)
            self.act(rs[:], rs[:], AF.Exp, [("ring", irs)], [("ring", irs)], scale=-0.5)
            for h in range(4):
                self.gate_out(l, h, slg, po[h], rs, irs, pv[:, nw0 + h:nw0 + h + 1], AF.Sigmoid, 8 + h)
            R.put(irs)
        for h in range(4):
            self.ps.put(po[h])
        self.slabs.put(slg)

    def gate_out(self, l, h, slg, pob, rs, irs, nw, func, yc):
        R = self.ring
        bg = self.proj_fm(slg, self.xT, "xT", h * P)
        isg = R.get()
        sg = self.ring_t[isg]
        self.act(sg[:], self.ps_t[bg][:, :], func, [("ps", bg)], [("ring", isg)])
        self.ps.put(bg)
        self.tt(sg[:], sg[:], rs[:], ALU.mult, [("ring", isg), ("ring", irs)], [("ring", isg)])
        self.stt(self.yT[:, yc, :], self.ps_t[pob][:, :], nw, sg[:], ALU.mult, ALU.mult, [("ps", pob), "pv", ("ring", isg)], [("yT", yc)])
        R.put(isg)

    def merge_out(self, l, t):
        R = self.ring
        for dh in range(2):
            slb = [self.slab_load(f"wbr{l}_{n}_{dh}", 4, 512) for n in range(3)]
            accs = []
            for dcl in range(4):
                ia = R.get()
                accs.append(ia)
            for n in range(3):
                pbs = []
                for dcl in range(4):
                    pb = self.ps.get()
                    st = self.slab_t[slb[n]]
                    pairs = [(st[:, kc, dcl * P:(dcl + 1) * P], self.yT[:, n * 4 + kc, :]) for kc in range(4)]
                    self.mmk(self.ps_t[pb][:, :], pairs, [("slab", slb[n])] + [("yT", n * 4 + kc) for kc in range(4)], [("ps", pb)])
                    pbs.append(pb)
                self.slabs.put(slb[n])
                slgt = self.slab_load(f"win{l}_gt{n * 2 + dh}", 8, 512)
                for dcl in range(4):
                    pg = self.proj_fm(slgt, self.xT, "xT", dcl * P)
                    isg = R.get()
                    sg = self.ring_t[isg]
                    self.act(sg[:], self.ps_t[pg][:, :], AF.Sigmoid, [("ps", pg)], [("ring", isg)])
                    self.ps.put(pg)
                    acc = self.ring_t[accs[dcl]]
                    ak = ("ring", accs[dcl])
                    if n == 0:
                        self.tt(acc[:], self.ps_t[pbs[dcl]][:, :], sg[:], ALU.mult, [("ps", pbs[dcl]), ("ring", isg)], [ak])
                    else:
                        self.tt(sg[:], self.ps_t[pbs[dcl]][:, :], sg[:], ALU.mult, [("ps", pbs[dcl]), ("ring", isg)], [("ring", isg)])
                        if n == 1:
                            self.tt(acc[:], acc[:], sg[:], ALU.add, [ak, ("ring", isg)], [ak])
                        else:
                            self.tt(self.mixT[:, dh * 4 + dcl, :], acc[:], sg[:], ALU.add, [ak, ("ring", isg)], [("mixT", dh * 4 + dcl)])
                    self.ps.put(pbs[dcl])
                    R.put(isg)
                self.slabs.put(slgt)
            for ia in accs:
                R.put(ia)
        for dh in range(2):
            slo = self.slab_load(f"wout{l}_{dh}", 8, 512)
            for blk in range(4):
                b = self.proj_tm(slo, self.mixT, "mixT", blk)
                xs = self.xtok[:, blk, dh * 512:(dh + 1) * 512]
                self.stt(xs, xs, ALPHA, self.ps_t[b][:, :], ALU.mult, ALU.add, [("ps", b), ("xtok", self.xi, blk)], [("xtok", self.xi, blk)])
                self.ps.put(b)
            self.slabs.put(slo)

    def layernorm(self, j0):
        for blk in range(4):
            xk = ("xtok", self.xi, blk)
            for hf in range(2):
                self.op("dve", lambda e, o=self.lnst[:, blk, hf, :], i_=self.xtok[:, blk, hf * 512:(hf + 1) * 512]: e.bn_stats(out=o, in_=i_),
                        [xk], [("lnst", blk)])
            self.op("dve", lambda e, o=self.lnmv[:, blk, :], i_=self.lnst[:, blk, :, :].rearrange("p a b -> p (a b)"): e.bn_aggr(out=o, in_=i_),
                    [("lnst", blk)], [("lnmv", blk)])
            self.act(self.lnr[:, blk, 0:1], self.lnmv[:, blk, 1:2], AF.Sqrt, [("lnmv", blk)], [("lnr", blk)], bias=LN_EPS)
            self.op("dve", lambda e, o=self.lnr[:, blk, 1:2], i_=self.lnr[:, blk, 0:1]: e.reciprocal(out=o, in_=i_), [("lnr", blk)], [("lnr", blk)])
            xs = self.xtok[:, blk, :]
            self.ts(xs, xs, self.lnmv[:, blk, 0:1], self.lnr[:, blk, 1:2], ALU.subtract, ALU.mult, [xk, ("lnmv", blk), ("lnr", blk)], [xk])
            self.tt(xs, xs, self.lnp[:, j0, :], ALU.mult, [xk, ("lnp", j0)], [xk], eng="pool")
            self.tt(xs, xs, self.lnp[:, j0 + 1, :], ALU.add, [xk, ("lnp", j0 + 1)], [xk], eng="pool")

    def up_proj(self, names_g, names_u, ngroups, nfc_of):
        R = self.ring
        fc = 0
        for g in range(ngroups):
            nf = nfc_of(g)
            sg_ = self.slab_load(names_g(g), 8, nf * P)
            su_ = self.slab_load(names_u(g), 8, nf * P)
            for j in range(nf):
                pg = self.proj_fm(sg_, self.x1T, "x1T", j * P)
                pu = self.proj_fm(su_, self.x1T, "x1T", j * P)
                isg = R.get()
                sg = self.ring_t[isg]
                self.act(sg[:], self.ps_t[pg][:, :], AF.Silu, [("ps", pg)], [("ring", isg)])
                self.ps.put(pg)
                self.tt(self.hT[:, fc, :], self.ps_t[pu][:, :], sg[:], ALU.mult, [("ps", pu), ("ring", isg)], [("hT", fc)])
                self.ps.put(pu)
                R.put(isg)
                fc += 1
            self.slabs.put(sg_)
            self.slabs.put(su_)
        return fc

    def down_proj(self, names_d, ngroups, nfc_of, evac):
        for dh in range(2):
            pbs = [self.ps.get() for _ in range(4)]
            fc = 0
            nfc_tot = sum(nfc_of(g) for g in range(ngroups))
            for g in range(ngroups):
                nf = nfc_of(g)
                sd = self.slab_load(names_d(g, dh), nf, 512)
                st = self.slab_t[sd]
                for j in range(nf):
                    for blk in range(4):
                        self.mm(self.ps_t[pbs[blk]][:, :], self.hT[:, fc, blk * P:(blk + 1) * P], st[:, j, :], fc == 0, fc == nfc_tot - 1,
                                [("hT", fc), ("slab", sd)], [("ps", pbs[blk])])
                    fc += 1
                self.slabs.put(sd)
            for blk in range(4):
                evac(blk, dh, pbs[blk])
                self.ps.put(pbs[blk])

    def ffn_dense(self):
        nfc = lambda g: 4 if g < 5 else 2
        self.up_proj(lambda g: f"fg_{g}", lambda g: f"fu_{g}", 6, nfc)

        def evac(blk, dh, pb):
            xs = self.xtok[:, blk, dh * 512:(dh + 1) * 512]
            self.stt(xs, xs, ALPHA, self.ps_t[pb][:, :], ALU.mult, ALU.add, [("ps", pb), ("xtok", self.xi, blk)], [("xtok", self.xi, blk)])
        self.down_proj(lambda g, dh: f"fd_{g}_{dh}", 6, nfc, evac)

    def moe(self):
        R = self.ring
        for blk in range(4):
            irt = [R.get(), R.get()]
            rtf = [self.ring_t[i][:].rearrange("p (k t) -> p k t", t=P) for i in irt]
            for half in range(2):
                b = self.ps.get()

                def fn(e, blk=blk, half=half, b=b, xt=self.xtok):
                    inst = None
                    for k4 in range(4):
                        kc = half * 4 + k4
                        inst = e.transpose(self.ps_t[b][:, k4 * P:(k4 + 1) * P], xt[:, blk, kc * P:(kc + 1) * P], self.consts[:, 0:128])
                    return inst
                self.op("pe", fn, [("xtok", self.xi, blk), "consts"], [("ps", b)])
                self.cp("act" if half else "dve", self.ring_t[irt[half]][:], self.ps_t[b][:, :], [("ps", b)], [("ring", irt[half])])
                self.ps.put(b)
            b = self.ps.get()
            pairs = [(rtf[kc // 4][:, kc % 4, :], self.rw[:, kc, :]) for kc in range(8)]
            self.mmk(self.ps_t[b][:, 0:8], pairs, [("ring", irt[0]), ("ring", irt[1]), "rw"], [("ps", b)])
            R.put(irt[0])
            R.put(irt[1])
            rl = self.rl[:, blk, :]
            rk = ("rl", blk)
            self.cp("dve", rl, self.ps_t[b][:, 0:8], [("ps", b)], [rk])
            self.ps.put(b)
            m8 = self.rm8[:, blk, :]
            self.op("dve", lambda e, o=m8, i_=rl: e.max(out=o, in_=i_), [rk], [("rm8", blk)])
            sm = self.rsm[:, blk, :]
            self.ts(sm[:, 0:1], m8[:, 0:1], -1.0, None, ALU.mult, None, [("rm8", blk)], [("rsm", blk)])
            wt = self.rwt[:, blk, :]
            self.act(wt, rl, AF.Exp, [rk, ("rsm", blk)], [("rwt", blk)], bias=sm[:, 0:1])
            self.ts(rl, rl, m8[:, 1:2], None, ALU.is_ge, None, [rk, ("rm8", blk)], [rk])
            self.tt(wt, wt, rl, ALU.mult, [("rwt", blk), rk], [("rwt", blk)])
            self.op("dve", lambda e, o=sm[:, 1:2], i_=wt: e.tensor_reduce(out=o, in_=i_, axis=AX.X, op=ALU.add), [("rwt", blk)], [("rsm", blk)])
            self.op("dve", lambda e, o=sm[:, 2:3], i_=sm[:, 1:2]: e.reciprocal(out=o, in_=i_), [("rsm", blk)], [("rsm", blk)])
            self.ts(wt, wt, sm[:, 2:3], None, ALU.mult, None, [("rwt", blk), ("rsm", blk)], [("rwt", blk)])
            xs = self.xtok[:, blk, :]
            self.ts(xs, xs, ALPHA, None, ALU.mult, None, [("xtok", self.xi, blk)], [("xtok", self.xi, blk)])
        nfc = lambda g: 4
        for ex in range(8):
            self.up_proj(lambda g: f"mg_{ex}_{g}", lambda g: f"mu_{ex}_{g}", 7, nfc)

            def evac(blk, dh, pb, ex=ex):
                xs = self.xtok[:, blk, dh * 512:(dh + 1) * 512]
                self.stt(xs, self.ps_t[pb][:, :], self.rwt[:, blk, ex:ex + 1], xs, ALU.mult, ALU.add,
                         [("ps", pb), ("xtok", self.xi, blk), ("rwt", blk)], [("xtok", self.xi, blk)])
            self.down_proj(lambda g, dh: f"md_{ex}_{g}_{dh}", 7, nfc, evac)


def _consts():
    c = np.zeros((P, 1024), np.float32)
    c[:, 0:128] = np.eye(P, dtype=np.float32)
    m = np.ones((P, 512), np.float32)
    m[:, 0::64] = 0.0
    c[:, 128:640] = m
    s = np.arange(P) % 64
    tcol = np.arange(256) % 64
    c[:, 640:896] = (s[:, None] <= tcol[None, :]).astype(np.float32)
    c[:, 896:1024] = 1.0
    return c


def _pvec(inp):
    def fm(v, nch):
        return np.ascontiguousarray(np.asarray(v, np.float32).reshape(nch, P).T)
    out = np.zeros((P, 2 * NPV), np.float32)
    for l in range(2):
        o = l * NPV
        cw = np.asarray(inp["conv_w"][l], np.float32)
        out[:, o:o + 16] = cw.reshape(4, 4, P).transpose(2, 1, 0).reshape(P, 16)
        out[:, o + 16:o + 20] = fm(inp["conv_b"][l], 4)
        out[:, o + 20:o + 24] = fm(inp["rg_br"][l], 4)
        out[:, o + 24:o + 28] = fm(inp["rg_bi"][l], 4)
        out[:, o + 28:o + 32] = fm(inp["rg_lambda"][l], 4)
        out[:, o + 32:o + 34] = fm(inp["gla_ba"][l], 2)
        out[:, o + 34:o + 35] = fm(inp["gla_norm_w"][l], 1)
        out[:, o + 35:o + 39] = fm(inp["hg_lb_logits"][0], 4)
        out[:, o + 39:o + 43] = fm(inp["hg_lb_logits"][1], 4)
        out[:, o + 43:o + 47] = fm(inp["hg_norm_w"][l], 4)
    return out


def _wbd(inp):
    w = np.zeros((2, 2, P, 4, P), np.float32)
    for l in range(2):
        for j, nm in enumerate(("rg_wr", "rg_wi")):
            a = np.asarray(inp[nm][l], np.float32)
            for g in range(8):
                c, h = g // 2, g % 2
                w[l, j, h * 64:(h + 1) * 64, c, h * 64:(h + 1) * 64] = a[g]
    return w


_PROGS = {}


DEBUG = None


def _prog(phases, mode):
    key = (tuple(phases), mode, str(DEBUG))
    if key not in _PROGS:
        _PROGS[key] = Builder(list(phases), mode, DEBUG).build()
    return _PROGS[key]


WNAMES = ("w_in", "gla_wa", "w_branch", "w_out", "ln1_g", "ln1_b", "ln2_g", "ln2_b", "ffn_wg", "ffn_wu", "ffn_wd",
          "router_w", "moe_wg", "moe_wu", "moe_wd")


def _base_maps(inp):
    x = np.ascontiguousarray(np.asarray(inp["x"], np.float32))
    consts = _consts()
    pvec = _pvec(inp)
    wbd = _wbd(inp)
    shared = {k: np.ascontiguousarray(np.asarray(inp[k], np.float32)) for k in WNAMES}
    maps = []
    for c in range(8):
        b, s = c // 4, c % 4
        xs = x[b, s * SEG:(s + 1) * SEG]
        halo = x[b, s * SEG - 4:s * SEG] if s > 0 else np.zeros((4, D), np.float32)
        mk = np.zeros((P, 8), np.float32)
        hm = np.zeros((P, 8), np.float32)
        for j in range(8):
            if j // 4 == b and j < c:
                mk[:, j] = 1.0
        if s > 0:
            hm[:, c - 1] = 1.0
        m = dict(shared)
        m.update({"x": np.ascontiguousarray(xs), "halo0": np.ascontiguousarray(halo), "masks": mk, "hmask": hm,
                  "consts": consts, "pvec": pvec, "wbd": wbd})
        maps.append(m)
    return maps


def _fused_maps(inp):
    x = np.asarray(inp["x"], np.float32)
    consts = _consts()
    pvec = _pvec(inp)
    wbd = _wbd(inp)
    shared = {k: np.ascontiguousarray(np.asarray(inp[k], np.float32)) for k in WNAMES}
    maps = []
    for c in range(8):
        b, s = c // 4, c % 4
        nreal = (s + 1) * SEG
        xl = np.zeros((NTF * TT, D), np.float32)
        xl[NTF * TT - nreal:] = x[b, 0:nreal]
        keep = np.zeros((P, NTF), np.float32)
        keep[:, (NTF * TT - nreal) // TT:] = 1.0
        m = dict(shared)
        m.update({"xlay": xl, "keep": keep, "consts": consts, "pvec": pvec, "wbd": wbd})
        maps.append(m)
    return maps


BIG = ("w_in", "w_branch", "w_out")
NEED = {"L0P1": ("w_in", "gla_wa", "router_w"),
        "L0P2": ("w_in", "gla_wa", "router_w", "w_branch", "w_out", "ln1_g", "ln1_b", "ln2_g", "ln2_b", "ffn_wg", "ffn_wu", "ffn_wd"),
        "L1P1": ("w_in", "gla_wa", "router_w"),
        "L1P2": ("w_in", "gla_wa", "router_w", "w_branch", "w_out", "ln1_g", "ln1_b", "ln2_g", "ln2_b", "moe_wg", "moe_wu", "moe_wd")}
COMMON = ("x", "halo0", "masks", "hmask", "consts", "pvec", "wbd")


def _run(phases, mode, maps, extra=()):
    nc = _prog(phases, mode)
    if mode == "fused":
        ms = maps
    else:
        p = phases[0]
        l = 0 if p.startswith("L0") else 1
        ms = []
        for m in maps:
            mm = {k: m[k] for k in COMMON if not (l == 1 and k in ("x", "halo0"))}
            for k in NEED[p]:
                mm[k] = np.ascontiguousarray(m[k][l:l + 1]) if k in BIG else m[k]
            for k in extra:
                mm[k] = m[k]
            ms.append(mm)
    res = run_bass_kernel_spmd(nc, ms, core_ids=list(range(8)))
    return res.results


MODE = "fused"


def kernel(**inp):
    if MODE == "fused":
        res = _run(["F"], "fused", _fused_maps(inp))
        y = np.stack([r["y"] for r in res]).reshape(2, 4 * SEG, D)
        return np.ascontiguousarray(y.astype(np.float32))
    maps = _base_maps(inp)
    rA = _run(["L0P1"], "multi", maps)
    st0_all = np.concatenate([r["st0"] for r in rA], axis=0)
    mB = [dict(m, st0_all=st0_all) for m in maps]
    rB = _run(["L0P2"], "multi", mB, extra=("st0_all",))
    r1 = [r["r1"] for r in rB]
    hl_all = np.concatenate([r[-4:] for r in r1], axis=0)
    mC = [dict(m, r1=r1[c], hl_all=hl_all) for c, m in enumerate(maps)]
    rC = _run(["L1P1"], "multi", mC, extra=("r1", "hl_all"))
    st1_all = np.concatenate([r["st1"] for r in rC], axis=0)
    mD = [dict(m, st1_all=st1_all) for m in mC]
    rD = _run(["L1P2"], "multi", mD, extra=("r1", "hl_all", "st1_all"))
    y = np.stack([r["y"] for r in rD]).reshape(2, 4 * SEG, D)
    return np.ascontiguousarray(y.astype(np.float32))
```
